# Optimizing a Trainium2 kernel written in Bass

```python
import jax, jax.numpy as jnp
from jax import lax
import numpy as np

D_MODEL = 1024
BATCH = 8
SEQ = 2048
DEPTH = 2

CHUNK = 64
D_MIX = D_MODEL
D_A = D_MIX // 2
D_B = D_MIX - D_A
GMLP_BLOCK = 128
N_HEADS_A = 8
HEAD_DIM_A = D_A // N_HEADS_A
CONV_WIDTH = 31
D_IN = 2 * D_A + 2 * D_B
N_EXPERTS = 8
TOP_K = 2
D_FF = 3584
N_DENSE = (DEPTH + 1) // 2
N_MOE = DEPTH // 2
EPS = 1e-6

kernel_name = "hybrid_gmlp_conformer_moe_block"


def rms_norm(x, g):
    x32 = x.astype(jnp.float32)
    y = x32 * lax.rsqrt(jnp.mean(x32 * x32, axis=-1, keepdims=True) + EPS)
    return y.astype(x.dtype) * g


def layer_norm(x, g, b):
    x32 = x.astype(jnp.float32)
    mu = jnp.mean(x32, axis=-1, keepdims=True)
    xc = x32 - mu
    y = xc * lax.rsqrt(jnp.mean(xc * xc, axis=-1, keepdims=True) + EPS)
    return y.astype(x.dtype) * g + b


def spatial_mask():
    cid = jnp.arange(GMLP_BLOCK) // CHUNK
    return cid[:, None] >= cid[None, :]


def gmlp_mixer(u, v, ln_g, ln_b, w_s, b_s):
    bsz, seq, _ = u.shape
    u = jax.nn.gelu(u)
    v = layer_norm(jax.nn.gelu(v), ln_g, ln_b)
    vb = v.reshape(bsz, seq // GMLP_BLOCK, GMLP_BLOCK, N_HEADS_A, HEAD_DIM_A)
    ws = jnp.where(spatial_mask()[None], w_s, jnp.zeros_like(w_s))
    mixed = jnp.einsum('hts,bnshd->bnthd', ws, vb) + b_s.T[:, :, None]
    return u * mixed.reshape(bsz, seq, D_A)


def conv_mixer(a, g, conv_w, conv_b, ln_g, ln_b):
    xg = a * jax.nn.sigmoid(g)
    y = lax.conv_general_dilated(
        xg, conv_w[:, None, :], window_strides=(1,),
        padding=[(CONV_WIDTH - 1, 0)],
        dimension_numbers=('NWC', 'WIO', 'NWC'),
        feature_group_count=D_B) + conv_b
    return jax.nn.silu(layer_norm(y, ln_g, ln_b))


def swiglu(h, wg, wu, wd):
    return (jax.nn.silu(h @ wg) * (h @ wu)) @ wd


def moe_swiglu(h, router_w, router_b, wg, wu, wd):
    logits = (h @ router_w).astype(jnp.float32) + router_b.astype(jnp.float32)
    top_vals, top_idx = lax.top_k(logits, TOP_K)
    top_w = jax.nn.softmax(top_vals, axis=-1)
    gates = jnp.sum(jax.nn.one_hot(top_idx, N_EXPERTS, dtype=jnp.float32) * top_w[..., None],
                    axis=-2).astype(h.dtype)
    out = jnp.zeros_like(h)
    for e in range(N_EXPERTS):
        out = out + gates[..., e:e + 1] * swiglu(h, wg[e], wu[e], wd[e])
    return out


def setup_inputs(seed: int = 0) -> dict:
    key = jax.random.key(seed)
    ks = jax.random.split(key, 24)
    f32 = jnp.float32
    nrm = lambda k, shape, scale: jax.random.normal(k, shape, f32) * scale
    return {
        "x": nrm(ks[0], (BATCH, SEQ, D_MODEL), 1.0),
        "c": nrm(ks[1], (BATCH, D_MODEL), 1.0),
        "w_ada": nrm(ks[2], (DEPTH, D_MODEL, 6 * D_MODEL), 0.5 * D_MODEL ** -0.5),
        "b_ada": nrm(ks[3], (DEPTH, 6 * D_MODEL), 0.02),
        "norm_gain": 1.0 + nrm(ks[4], (DEPTH, 4, D_MODEL), 0.05),
        "w_in": nrm(ks[5], (DEPTH, D_MODEL, D_IN), D_MODEL ** -0.5),
        "b_in": nrm(ks[6], (DEPTH, D_IN), 0.02),
        "ln_v_gain": 1.0 + nrm(ks[7], (DEPTH, D_A), 0.05),
        "ln_v_bias": nrm(ks[8], (DEPTH, D_A), 0.02),
        "w_spatial": nrm(ks[9], (DEPTH, N_HEADS_A, GMLP_BLOCK, GMLP_BLOCK), GMLP_BLOCK ** -0.5),
        "b_spatial": 1.0 + nrm(ks[10], (DEPTH, N_HEADS_A, GMLP_BLOCK), 0.05),
        "conv_w": nrm(ks[11], (DEPTH, CONV_WIDTH, D_B), CONV_WIDTH ** -0.5),
        "conv_b": nrm(ks[12], (DEPTH, D_B), 0.02),
        "ln_conv_gain": 1.0 + nrm(ks[13], (DEPTH, D_B), 0.05),
        "ln_conv_bias": nrm(ks[14], (DEPTH, D_B), 0.02),
        "group_gain": 1.0 + nrm(ks[15], (DEPTH, D_MIX), 0.05),
        "w_out": nrm(ks[16], (DEPTH, D_MIX, D_MODEL), D_MIX ** -0.5),
        "ffn_w_gate": nrm(ks[17], (N_DENSE, D_MODEL, D_FF), D_MODEL ** -0.5),
        "ffn_w_up": nrm(ks[18], (N_DENSE, D_MODEL, D_FF), D_MODEL ** -0.5),
        "ffn_w_down": nrm(ks[19], (N_DENSE, D_FF, D_MODEL), D_FF ** -0.5),
        "router_w": nrm(ks[20], (N_MOE, D_MODEL, N_EXPERTS), D_MODEL ** -0.5),
        "router_b": nrm(ks[21], (N_MOE, N_EXPERTS), 0.01),
        "moe_w_gate": nrm(ks[22], (N_MOE, N_EXPERTS, D_MODEL, D_FF), D_MODEL ** -0.5),
        "moe_w_up": nrm(jax.random.fold_in(ks[23], 0), (N_MOE, N_EXPERTS, D_MODEL, D_FF), D_MODEL ** -0.5),
        "moe_w_down": nrm(jax.random.fold_in(ks[23], 1), (N_MOE, N_EXPERTS, D_FF, D_MODEL), D_FF ** -0.5),
    }


def reference(x, c, w_ada, b_ada, norm_gain, w_in, b_in, ln_v_gain, ln_v_bias, w_spatial,
              b_spatial, conv_w, conv_b, ln_conv_gain, ln_conv_bias, group_gain, w_out,
              ffn_w_gate, ffn_w_up, ffn_w_down, router_w, router_b, moe_w_gate, moe_w_up,
              moe_w_down):
    c_act = jax.nn.silu(c)
    for l in range(DEPTH):
        mod = (c_act @ w_ada[l] + b_ada[l])[:, None, :]
        sh1, sc1, g1, sh2, sc2, g2 = jnp.split(mod, 6, axis=-1)

        h = rms_norm(x, norm_gain[l, 0]) * (1.0 + sc1) + sh1
        z = h @ w_in[l] + b_in[l]
        ua, va, ab, gb = jnp.split(z, [D_A, 2 * D_A, 2 * D_A + D_B], axis=-1)
        ya = gmlp_mixer(ua, va, ln_v_gain[l], ln_v_bias[l], w_spatial[l], b_spatial[l])
        yb = conv_mixer(ab, gb, conv_w[l], conv_b[l], ln_conv_gain[l], ln_conv_bias[l])
        y = jnp.concatenate([rms_norm(ya, group_gain[l, :D_A]),
                             rms_norm(yb, group_gain[l, D_A:])], axis=-1) @ w_out[l]
        x = x + g1 * rms_norm(y, norm_gain[l, 1])

        h = rms_norm(x, norm_gain[l, 2]) * (1.0 + sc2) + sh2
        if l % 2 == 0:
            i = l // 2
            f = swiglu(h, ffn_w_gate[i], ffn_w_up[i], ffn_w_down[i])
        else:
            i = l // 2
            f = moe_swiglu(h, router_w[i], router_b[i], moe_w_gate[i], moe_w_up[i], moe_w_down[i])
        x = x + g2 * rms_norm(f, norm_gain[l, 3])
    return x
```

```python
from contextlib import ExitStack

import numpy as np
import concourse.bass as bass
import concourse.mybir as mybir
from concourse.bass_utils import run_bass_kernel_spmd

F32 = mybir.dt.float32
BF16 = mybir.dt.bfloat16
AF = mybir.ActivationFunctionType
ALU = mybir.AluOpType
AX = mybir.AxisListType

D = 1024
T = 2048
KC = 8
DFF = 3584
NFC = 28
NE = 8
EPS = 1e-6
MT = 256
NMT = T // MT
FT = 512
NFT = T // FT
NPL = 236
COMPUTE = ("pe", "act", "dve", "pool")


class Op:
    __slots__ = ("eng", "fn", "deps", "sig", "val", "semkey", "nosync", "gi")


class Sched:
    def __init__(self):
        self.ops = {e: [] for e in ("pe", "act", "dve", "pool", "sp")}
        self.lastw = {}
        self.readers = {}
        self.n = 0
        self.dmas = []

    def add(self, eng, fn, reads=(), writes=(), dma=None, nosync=False):
        op = Op()
        op.eng = eng
        op.fn = fn
        op.sig = False
        op.val = None
        op.semkey = dma
        op.nosync = nosync
        op.gi = self.n
        deps = {}
        for k in reads:
            w = self.lastw.get(k)
            if w is not None:
                deps[id(w)] = w
        for k in writes:
            w = self.lastw.get(k)
            if w is not None:
                deps[id(w)] = w
            rd = self.readers.get(k)
            if rd:
                for r in rd.values():
                    deps[id(r)] = r
        op.deps = list(deps.values())
        for k in writes:
            self.lastw[k] = op
            self.readers[k] = {}
        for k in reads:
            rk = eng if dma is None else ("dma", self.n)
            self.readers.setdefault(k, {})[rk] = op
        self.ops[eng].append(op)
        if dma is not None:
            self.dmas.append(op)
        self.n += 1
        return op

    def barrier(self):
        lasts = []
        for lst in self.ops.values():
            for op in reversed(lst):
                if op.semkey is None and op.fn is not None:
                    lasts.append(op)
                    break
        deps = lasts + self.dmas
        for e in self.ops:
            op = Op()
            op.eng = e
            op.fn = None
            op.sig = False
            op.val = None
            op.semkey = None
            op.nosync = False
            op.gi = self.n
            op.deps = [d for d in deps]
            self.ops[e].append(op)
        self.dmas = []
        self.lastw = {}
        self.readers = {}

    def emit(self, nc, final_wait_ops=()):
        for e, lst in self.ops.items():
            for op in lst:
                for d in op.deps:
                    if d.semkey is None:
                        if d.eng == op.eng and op.semkey is None and op.nosync:
                            continue
                        d.sig = True
        cnt = {e: 0 for e in COMPUTE}
        dcnt = {}
        for e, lst in self.ops.items():
            for op in lst:
                if op.semkey is not None:
                    dcnt[op.semkey] = dcnt.get(op.semkey, 0) + 16
                    op.val = dcnt[op.semkey]
                elif op.sig:
                    cnt[op.eng] += 1
                    op.val = cnt[op.eng]
        with ExitStack() as st:
            sems = {e: st.enter_context(nc.semaphore("s_" + e)) for e in COMPUTE}
            dsems = {k: st.enter_context(nc.semaphore("d_" + str(i))) for i, k in enumerate(dcnt)}
            block = st.enter_context(nc.Block())
            sched = self

            def needs_of(op):
                need = {}
                for d in op.deps:
                    if d.semkey is not None:
                        s = dsems[d.semkey]
                    else:
                        if not d.sig:
                            continue
                        if d.eng == op.eng and op.semkey is None and op.nosync:
                            continue
                        s = sems[d.eng]
                    cur = need.get(s.num)
                    if cur is None or cur[1] < d.val:
                        need[s.num] = (s, d.val, d.gi)
                return need

            def run(eng_name, eng):
                known = {}
                lst = sched.ops[eng_name]
                needs = [needs_of(op) for op in lst]
                look = 12 if eng_name == "pe" else 0
                for j, op in enumerate(lst):
                    for num, (s, v, _) in needs[j].items():
                        if known.get(num, 0) < v:
                            vv = v
                            for jj in range(j + 1, min(len(lst), j + 1 + look)):
                                nx = needs[jj].get(num)
                                if nx is not None and nx[2] < op.gi and nx[1] > vv:
                                    vv = nx[1]
                            eng.wait_ge(s, vv)
                            known[num] = vv
                    if op.fn is None:
                        continue
                    ins = op.fn(eng)
                    if op.semkey is not None:
                        ins.then_inc(dsems[op.semkey], 16)
                    elif op.sig:
                        ins.then_inc(sems[op.eng], 1)
                if eng_name == "sp":
                    for op in final_wait_ops:
                        eng.wait_ge(dsems[op.semkey], op.val)

            block.sync(lambda e: run("sp", e))
            block.scalar(lambda e: run("act", e))
            block.vector(lambda e: run("dve", e))
            block.gpsimd(lambda e: run("pool", e))
            block.tensor(lambda e: run("pe", e))


class Arena:
    def __init__(self, ap, nbytes):
        self.ap = ap
        self.nbytes = nbytes
        self.off = 0

    def reset(self):
        self.off = 0

    def alloc(self, free_shape, dtype):
        esz = 4 if dtype == F32 else 2
        n = int(np.prod(free_shape)) * esz
        n_al = (n + 31) // 32 * 32
        assert self.off + n_al <= self.nbytes, ("arena overflow", self.off, n_al, self.nbytes)
        v = self.ap[:, self.off // 2:(self.off + n) // 2]
        self.off += n_al
        if dtype == F32:
            v = v.bitcast(F32)
        if len(free_shape) == 2:
            v = v.rearrange("p (a b) -> p a b", b=free_shape[1])
        elif len(free_shape) == 3:
            v = v.rearrange("p (a b c) -> p a b c", b=free_shape[1], c=free_shape[2])
        return v


def build_program(n_layers=2, stop_after=None):
    nc = bass.Bass("TRN2", target_bir_lowering=False)

    def din(name, shape):
        return nc.dram_tensor(name, list(shape), F32, kind="ExternalInput").ap()

    xT_d = din("xT", [D, T])
    cT_d = din("cT", [128, KC])
    pcols_d = din("pcols", [128, 2 * NPL])
    ident_d = din("ident", [128, 128])
    w_ada_d = din("w_ada", [2, D, 6 * D])
    w_in_d = din("w_in", [2, D, 2 * D])
    w_out_d = din("w_out", [2, D, D])
    b_in_row_d = din("b_in_row", [2, D])
    rowvecs_d = din("rowvecs", [2, 3, 512])
    wsT_d = din("wsT", [2, 128, 8, 128])
    ffn_wg_d = din("ffn_wg", [1, D, DFF])
    ffn_wu_d = din("ffn_wu", [1, D, DFF])
    ffn_wd_d = din("ffn_wd", [1, DFF, D])
    rw_d = din("router_w", [D, NE])
    rb_d = din("router_b", [NE])
    moe_wg_d = din("moe_wg", [NE, D, DFF])
    moe_wu_d = din("moe_wu", [NE, D, DFF])
    moe_wd_d = din("moe_wd", [NE, DFF, D])
    out_d = nc.dram_tensor("outT", [D, T], F32, kind="ExternalOutput").ap()

    ARENA_BYTES = 132 * 1024
    with ExitStack() as st:
        def sb(name, shape, dt):
            return st.enter_context(nc.sbuf_tensor(name, list(shape), dt))

        xT = sb("xT_sb", [128, KC, T], F32)
        pcols = sb("pcols_sb", [128, 2 * NPL], F32)
        modt = sb("modt", [128, 2, 48], F32)
        dcols = sb("dcols", [128, 2, 32], F32)
        ident32 = sb("ident32", [128, 128], F32)
        identbf = sb("identbf", [128, 128], BF16)
        onesD = sb("onesD", [128, 128], BF16)
        ones512 = sb("ones512", [128, 128], BF16)
        ones32_512 = sb("ones32_512", [128, 128], F32)
        ones32 = sb("ones32", [128, 128], F32)
        onesrow = sb("onesrow", [1, 128], BF16)
        epsc = sb("epsc", [128, 1], F32)
        cwh = sb("cwh", [128, 124], F32)
        eps4 = sb("eps4", [128, 1], F32)
        cst = sb("cst", [128, KC], F32)
        cact = sb("cact", [128, KC], BF16)
        small = sb("small", [128, 64], F32)
        lg = sb("lg", [128, 16, NE], F32)
        lg2 = sb("lg2", [128, 16, NE], F32)
        eq1 = sb("eq1", [128, 16, NE], F32)
        eq2 = sb("eq2", [128, 16, NE], F32)
        gates = sb("gates", [128, 16, NE], F32)
        rt = sb("rt", [128, 6, 16], F32)
        rw32 = sb("rw32", [128, KC, NE], F32)
        rb_bc = sb("rb_bc", [128, NE], F32)
        arena_t = sb("arena", [128, ARENA_BYTES // 2], BF16)
        ps = st.enter_context(nc.psum_tensor("ps", [128, 8, 512], F32))
        A = Arena(arena_t[:], ARENA_BYTES)
        S = Sched()

        def ACT(out, in_, func, r, w, bias=None, scale=None, accum=None, nosync=False):
            kw = {}
            if bias is not None:
                kw["bias"] = bias
            if scale is not None:
                kw["scale"] = scale
            if accum is not None:
                kw["accum_out"] = accum
            return S.add("act", lambda e: e.activation(out=out, in_=in_, func=func, **kw), r, w, nosync=nosync)

        def TT(eng, out, in0, in1, op, r, w, nosync=False):
            return S.add(eng, lambda e: e.tensor_tensor(out=out, in0=in0, in1=in1, op=op), r, w, nosync=nosync)

        def STT(eng, out, in0, scalar, in1, op0, op1, r, w, nosync=False):
            return S.add(eng, lambda e: e.scalar_tensor_tensor(out=out, in0=in0, scalar=scalar, in1=in1, op0=op0, op1=op1), r, w, nosync=nosync)

        def TS(eng, out, in0, s1, s2, op0, op1, r, w, nosync=False):
            if s2 is None:
                return S.add(eng, lambda e: e.tensor_scalar(out=out, in0=in0, scalar1=s1, scalar2=None, op0=op0), r, w, nosync=nosync)
            return S.add(eng, lambda e: e.tensor_scalar(out=out, in0=in0, scalar1=s1, scalar2=s2, op0=op0, op1=op1), r, w, nosync=nosync)

        def MM(out, lhsT, rhs, start, stop, r, w):
            return S.add("pe", lambda e: e.matmul(out, lhsT, rhs, start=start, stop=stop), r, w, nosync=True)

        def DMA(eng, out, in_, r, w, key):
            return S.add(eng, lambda e: e.dma_start(out=out, in_=in_), r, w, dma=key)

        def MEMSET(eng, ap, val, w):
            return S.add(eng, lambda e: e.memset(ap, val), (), w)

        def rsqrt_tile(out_sb, in_ps, r, w, wkey_tmp):
            ACT(out_sb, in_ps, AF.Sqrt, r + ["epsc"], w, bias=epsc[:, 0:1])
            S.add("dve", lambda e: e.reciprocal(out=out_sb, in_=out_sb), w, w)

        DMA("sp", pcols[:], pcols_d, [], ["pcols"], "pcols")
        DMA("sp", cst[:], cT_d, [], ["cst"], "cst")
        DMA("sp", ident32[:], ident_d, [], ["ident32"], "ident")
        for k in range(KC):
            DMA("sp", xT[:, k, :], xT_d[k * 128:(k + 1) * 128, :], [], [("x", k, mt) for mt in range(NMT)], ("xin", k))
        MEMSET("dve", onesD[:], 1.0 / D, ["onesD"])
        MEMSET("dve", ones512[:], 1.0 / 512, ["ones512"])
        MEMSET("dve", ones32_512[:], 1.0 / 512, ["ones32_512"])
        MEMSET("dve", ones32[:], 1.0, ["ones32"])
        MEMSET("dve", onesrow[:], 1.0, ["onesrow"])
        MEMSET("dve", epsc[:], EPS, ["epsc"])
        MEMSET("dve", eps4[:], 4.0 * EPS, ["eps4"])
        ACT(identbf[:], ident32[:], AF.Copy, ["ident32"], ["identbf"])
        ACT(cact[:], cst[:], AF.Silu, ["cst"], ["cact"])

        def pc(l, c0, n=1):
            return pcols[:, l * NPL + c0:l * NPL + c0 + n]

        def mod_dma(l, pi, stage):
            ns = len(stage)
            sl = stage[pi % ns]
            DMA("pool", sl, w_ada_d[l, :, pi * 256:(pi + 1) * 256].rearrange("(k p) n -> p k n", p=128), [], [("wada", pi % ns)], ("wada", pi % ns))

        def mod_mm(l, pi, stage):
            ns = len(stage)
            sl = stage[pi % ns]
            for o in range(2):
                oc = pi * 2 + o
                for k in range(KC):
                    MM(ps[:, 5, 256 + oc:256 + oc + 1], sl[:, k, o * 128:(o + 1) * 128], cact[:, k:k + 1], k == 0, k == KC - 1,
                       [("wada", pi % ns), "cact"], ["psmod"])

        def mod_fin(l):
            TT("dve", modt[:, l, :], ps[:, 5, 256:304], pc(l, 0, 48), ALU.add, ["psmod", "pcols"], [("mod", l)])
            STT("dve", dcols[:, l, 0:8], modt[:, l, 8:16], 1.0, pc(l, 48, 8), ALU.add, ALU.mult, [("mod", l), "pcols"], [("dc", l, 0)])
            TT("dve", dcols[:, l, 8:16], modt[:, l, 16:24], pc(l, 56, 8), ALU.mult, [("mod", l), "pcols"], [("dc", l, 1)])
            STT("dve", dcols[:, l, 16:24], modt[:, l, 32:40], 1.0, pc(l, 64, 8), ALU.add, ALU.mult, [("mod", l), "pcols"], [("dc", l, 2)])
            TT("dve", dcols[:, l, 24:32], modt[:, l, 40:48], pc(l, 72, 8), ALU.mult, [("mod", l), "pcols"], [("dc", l, 3)])

        def norm_mod_tile(l, which, c0, ncol, sq_bufs, rstd_buf, tmp_bufs, out_fn, xkeys_fn, stat_bank, h32_bufs=None, post=None):
            gsc = 0 if which == 1 else 16
            shc = 0 if which == 1 else 24
            for k in range(KC):
                sq = sq_bufs[k % 2]
                ACT(sq, xT[:, k, c0:c0 + ncol], AF.Square, [xkeys_fn(k)], [("sqA", k % 2)])
                MM(ps[:, stat_bank, 0:ncol], onesD[:], sq, k == 0, k == KC - 1, [("sqA", k % 2), "onesD"], [("ps", stat_bank)])
            rsqrt_tile(rstd_buf, ps[:, stat_bank, 0:ncol], [("ps", stat_bank)], ["rstdA"], "rstdA_t")
            for k in range(KC):
                tmp = tmp_bufs[k % 2]
                TT("dve", tmp, xT[:, k, c0:c0 + ncol], rstd_buf, ALU.mult, [xkeys_fn(k), "rstdA"], [("tmpA", k % 2)])
                dst, dkeys = out_fn(k)
                if h32_bufs is None:
                    ACT(dst, tmp, AF.Identity, [("tmpA", k % 2), ("dc", l, 0 if which == 1 else 2), ("mod", l)], dkeys,
                        scale=dcols[:, l, gsc + k:gsc + k + 1], bias=modt[:, l, shc + k:shc + k + 1])
                else:
                    h32 = h32_bufs[k % 2]
                    ACT(h32, tmp, AF.Identity, [("tmpA", k % 2), ("dc", l, 2), ("mod", l)], [("h32", k % 2)],
                        scale=dcols[:, l, gsc + k:gsc + k + 1], bias=modt[:, l, shc + k:shc + k + 1])
                    S.add("pool", lambda e, dst=dst, h32=h32: e.tensor_copy(out=dst, in_=h32), [("h32", k % 2)], dkeys)
                    post(k, h32, ("h32", k % 2))

        def mixer(l, do_mod_next):
            A.reset()
            w_in = A.alloc([KC, 2 * D], BF16)
            w_out = A.alloc([KC, D], BF16)
            hb = A.alloc([KC, MT], BF16)
            ycat = [A.alloc([KC, MT], BF16) for _ in range(3)]
            wsT = A.alloc([8, 128], BF16)
            bct = A.alloc([3, 512], F32)
            binrow = A.alloc([D], BF16)
            ub = [A.alloc([512], F32) for _ in range(2)]
            gv = A.alloc([512], F32)
            vb = [A.alloc([512], BF16) for _ in range(2)]
            yanb = [A.alloc([512], BF16) for _ in range(2)]
            junk = A.alloc([512], BF16)
            sg = A.alloc([MT], F32)
            abb = junk.bitcast(F32)
            xg = [A.alloc([4, 30 + MT], F32) for _ in range(2)]
            ycb = [A.alloc([4, MT], F32) for _ in range(2)]
            sqc = [A.alloc([MT], F32) for _ in range(2)]
            mean_sb = A.alloc([MT], F32)
            m2 = A.alloc([MT], F32)
            sqA = [A.alloc([MT], BF16) for _ in range(2)]
            sqB = [A.alloc([MT], BF16) for _ in range(2)]
            rstdA = A.alloc([MT], F32)
            tmpA = [A.alloc([MT], F32) for _ in range(2)]
            yo = A.alloc([KC, MT], F32)
            sqF = [A.alloc([MT], BF16) for _ in range(2)]
            rsF = A.alloc([MT], F32)
            tmpF = [A.alloc([MT], F32) for _ in range(2)]
            stage = [A.alloc([KC, 256], BF16) for _ in range(2)]

            DMA("pool", w_in, w_in_d[l].rearrange("(k p) n -> p k n", p=128), [], ["w_in"], "w_in")
            DMA("pool", wsT, wsT_d[l], [], ["wsT_raw"], "wsT")
            DMA("pool", binrow[0:1, :], b_in_row_d[l:l + 1, :], [], ["binrow"], "binrow")
            DMA("sp", bct, rowvecs_d[l].partition_broadcast(128), [], ["bct"], "bct")
            DMA("pool", w_out, w_out_d[l].rearrange("(k p) n -> p k n", p=128), [], ["w_out"], "w_out")
            MEMSET("dve", wsT[64:128, :, 0:64], 0.0, ["wsT_raw"])
            MEMSET("dve", xg[0][:, :, 0:30], 0.0, [("xg_halo", 0)])
            TS("dve", small[:, 32:36], pc(l, 84, 4), 0.5, None, ALU.mult, None, ["pcols"], ["hbcol"])
            TS("dve", cwh[:], pc(l, 104, 124), 0.5, None, ALU.mult, None, ["pcols"], ["cwh"])
            zi = [0]
            fi = [0]

            def zbank():
                zi[0] += 1
                return zi[0] % 2

            def fbank():
                fi[0] += 1
                return 2 + fi[0] % 2

            NB = MT // 128

            def rsq(out_sb, in_ap, r, w, e4=False):
                ACT(out_sb, in_ap, AF.Sqrt, r + ["epsc", "eps4"], w, bias=(eps4 if e4 else epsc)[:, 0:1])
                S.add("dve", lambda e: e.reciprocal(out=out_sb, in_=out_sb), w, w)

            def front(mt):
                c0 = mt * MT
                yb = ycat[mt % 3]
                xgb = xg[mt % 2]
                hk = lambda k: ("hT", k)
                if do_mod_next:
                    mod_dma(l + 1, 3 * mt, stage)
                    mod_dma(l + 1, 3 * mt + 1, stage)
                for k in range(KC):
                    sq = sqA[k % 2]
                    ACT(sq, xT[:, k, c0:c0 + MT], AF.Square, [("x", k, mt)], [("sqA", k % 2)])
                    MM(ps[:, 4, 0:MT], onesD[:], sq, k == 0, k == KC - 1, [("sqA", k % 2), "onesD"], [("ps", 4)])
                    yield
                rsq(rstdA, ps[:, 4, 0:MT], [("ps", 4)], ["rstdA"])
                yield
                for k in range(KC):
                    tmp = tmpA[k % 2]
                    TT("dve", tmp, xT[:, k, c0:c0 + MT], rstdA, ALU.mult, [("x", k, mt), "rstdA"], [("tmpA", k % 2)])
                    ACT(hb[:, k, :], tmp, AF.Identity, [("tmpA", k % 2), ("dc", l, 0), ("mod", l)], [hk(k)],
                        scale=dcols[:, l, k:k + 1], bias=modt[:, l, k:k + 1])
                    yield
                for bi in range(NB):
                    tb = mt * NB + bi
                    b0 = bi * 128
                    pb = tb % 2
                    u = ub[pb]
                    v = vb[pb]
                    yan = yanb[pb]
                    bu = zbank()
                    for k in range(KC):
                        MM(ps[:, bu, :], hb[:, k, b0:b0 + 128], w_in[:, k, 0:512], k == 0, False, [hk(k), "w_in"], [("ps", bu)])
                    MM(ps[:, bu, :], onesrow[0:1, :], binrow[0:1, 0:512], False, True, ["onesrow", "binrow"], [("ps", bu)])
                    yield
                    bv = zbank()
                    for k in range(KC):
                        MM(ps[:, bv, :], hb[:, k, b0:b0 + 128], w_in[:, k, 512:1024], k == 0, False, [hk(k), "w_in"], [("ps", bv)])
                    MM(ps[:, bv, :], onesrow[0:1, :], binrow[0:1, 512:1024], False, True, ["onesrow", "binrow"], [("ps", bv)])
                    yield
                    ACT(u, ps[:, bu, :], AF.Gelu_apprx_tanh, [("ps", bu)], [("u", pb)])
                    yield
                    ACT(gv, ps[:, bv, :], AF.Gelu_apprx_tanh, [("ps", bv)], ["gv"])
                    yield
                    s0 = pb * 16
                    S.add("dve", lambda e, s0=s0: e.bn_stats(out=small[:, s0:s0 + 6], in_=gv), ["gv"], [("st6", pb)])
                    S.add("dve", lambda e, s0=s0: e.bn_aggr(out=small[:, s0 + 6:s0 + 8], in_=small[:, s0:s0 + 6]), [("st6", pb)], [("mv", pb)])
                    yield
                    ACT(small[:, s0 + 8:s0 + 9], small[:, s0 + 7:s0 + 8], AF.Sqrt, [("mv", pb), "epsc"], [("sd", pb)], bias=epsc[:, 0:1])
                    S.add("dve", lambda e, s0=s0: e.reciprocal(out=small[:, s0 + 9:s0 + 10], in_=small[:, s0 + 8:s0 + 9]), [("sd", pb)], [("rv", pb)])
                    yield
                    STT("dve", small[:, s0 + 10:s0 + 11], small[:, s0 + 6:s0 + 7], -1.0, small[:, s0 + 9:s0 + 10], ALU.mult, ALU.mult,
                        [("mv", pb), ("rv", pb)], [("nb", pb)])
                    yield
                    ACT(gv, gv, AF.Identity, ["gv", ("rv", pb), ("nb", pb)], ["gv"],
                        scale=small[:, s0 + 9:s0 + 10], bias=small[:, s0 + 10:s0 + 11])
                    yield
                    TT("dve", gv, gv, bct[:, 0, :], ALU.mult, ["gv", "bct"], ["gv"])
                    yield
                    TT("dve", v, gv, bct[:, 1, :], ALU.add, ["gv", "bct"], [("v", pb)])
                    yield
                    bm = 4
                    for h in range(8):
                        MM(ps[:, bm, h * 64:(h + 1) * 64], wsT[:, h, :], v[:, h * 64:(h + 1) * 64], True, True,
                           ["wsT_raw", ("v", pb)], [("ps", bm)])
                    yield
                    for h in range(8):
                        STT("dve", u[:, h * 64:(h + 1) * 64], ps[:, bm, h * 64:(h + 1) * 64], pc(l, 228 + h), u[:, h * 64:(h + 1) * 64],
                            ALU.add, ALU.mult, [("ps", bm), ("u", pb), "pcols"], [("u", pb)], nosync=(h > 0))
                        if h % 2 == 1:
                            yield
                    MEMSET("dve", small[:, s0 + 11:s0 + 12], 0.0, [("ssA", pb)])
                    ACT(junk, u, AF.Square, [("u", pb), ("ssA", pb)], ["junk", ("ssA", pb)], accum=small[:, s0 + 11:s0 + 12])
                    yield
                    TS("dve", small[:, s0 + 12:s0 + 13], small[:, s0 + 11:s0 + 12], 1.0 / 512, EPS, ALU.mult, ALU.add, [("ssA", pb)], [("ssA2", pb)])
                    ACT(small[:, s0 + 13:s0 + 14], small[:, s0 + 12:s0 + 13], AF.Sqrt, [("ssA2", pb)], [("sdA", pb)])
                    S.add("dve", lambda e, s0=s0: e.reciprocal(out=small[:, s0 + 14:s0 + 15], in_=small[:, s0 + 13:s0 + 14]), [("sdA", pb)], [("rsA", pb)])
                    yield
                    STT("dve", yan, u, small[:, s0 + 14:s0 + 15], bct[:, 2, :], ALU.mult, ALU.mult, [("u", pb), ("rsA", pb), "bct"], [("yan", pb)])
                    yield
                    psT = ps[:, 5, 0:256].bitcast(BF16).rearrange("p (j t) -> p j t", t=128)
                    for j in range(4):
                        S.add("pe", lambda e, j=j, yan=yan, psT=psT: e.transpose(out=psT[:, j, :], in_=yan[:, j * 128:(j + 1) * 128], identity=identbf[:]),
                              [("yan", pb), "identbf"], [("ps", 5)], nosync=True)
                    ACT(yb[:, 0:4, b0:b0 + 128], psT, AF.Copy, [("ps", 5)], [("yc", mt % 3, j, bi) for j in range(4)])
                    yield
                if mt > 0:
                    S.add("pool", lambda e, xgb=xgb, prev=xg[(mt - 1) % 2]: e.tensor_copy(out=xgb[:, :, 0:30], in_=prev[:, :, MT:MT + 30]),
                          [("xg", (mt - 1) % 2, j) for j in range(4)], [("xg_halo", mt % 2)])
                for j in range(4):
                    ba = zbank()
                    for k in range(KC):
                        MM(ps[:, ba, 0:MT], w_in[:, k, (8 + j) * 128:(9 + j) * 128], hb[:, k, :], k == 0, k == KC - 1, [hk(k), "w_in"], [("ps", ba)])
                    yield
                    bg = zbank()
                    for k in range(KC):
                        MM(ps[:, bg, 0:MT], w_in[:, k, (12 + j) * 128:(13 + j) * 128], hb[:, k, :], k == 0, k == KC - 1, [hk(k), "w_in"], [("ps", bg)])
                    yield
                    ACT(sg, ps[:, bg, 0:MT], AF.Tanh, [("ps", bg), "hbcol"], ["sg"], scale=0.5, bias=small[:, 32 + j:33 + j])
                    ACT(abb, ps[:, ba, 0:MT], AF.Identity, [("ps", ba), "pcols"], ["junk"], bias=pc(l, 80 + j))
                    yield
                    STT("dve", xgb[:, j, 30:30 + MT], sg, 1.0, abb, ALU.add, ALU.mult,
                        ["junk", "sg"], [("xg", mt % 2, j)])
                    yield
                if do_mod_next:
                    mod_mm(l + 1, 3 * mt, stage)
                    yield
                    mod_dma(l + 1, 3 * mt + 2, stage)
                    mod_mm(l + 1, 3 * mt + 1, stage)
                    yield
                    mod_mm(l + 1, 3 * mt + 2, stage)
                    yield

            def conv(mt):
                xgb = xg[mt % 2]
                yc = ycb[mt % 2]
                xr = lambda j: [("xg", mt % 2, j), ("xg_halo", mt % 2)]
                for j in range(4):
                    TS("dve", yc[:, j, :], xgb[:, j, 0:MT], cwh[:, j:j + 1], pc(l, 88 + j), ALU.mult, ALU.add,
                       xr(j) + ["pcols", "cwh"], [("ycv", mt % 2, j)])
                    yield
                    for tap in range(1, 31):
                        STT("dve", yc[:, j, :], xgb[:, j, tap:tap + MT], cwh[:, tap * 4 + j:tap * 4 + j + 1], yc[:, j, :], ALU.mult, ALU.add,
                            xr(j) + [("ycv", mt % 2, j)], [("ycv", mt % 2, j)], nosync=True)
                        yield

            def post(mt):
                c0 = mt * MT
                yb = ycat[mt % 3]
                yc = ycb[mt % 2]
                yk = lambda j: ("ycv", mt % 2, j)
                for j in range(4):
                    MM(ps[:, 6, 0:MT], ones32_512[:], yc[:, j, :], j == 0, j == 3, [yk(j), "ones32_512"], [("ps", 6)])
                yield
                for j in range(4):
                    ACT(sqc[j % 2], yc[:, j, :], AF.Square, [yk(j)], [("sqc", j % 2)])
                    MM(ps[:, 7, 0:MT], ones32_512[:], sqc[j % 2], j == 0, j == 3, [("sqc", j % 2), "ones32_512"], [("ps", 7)])
                    yield
                ACT(mean_sb, ps[:, 6, 0:MT], AF.Copy, [("ps", 6)], ["mean_sb"])
                yield
                TT("dve", m2, mean_sb, mean_sb, ALU.mult, ["mean_sb"], ["m2"])
                yield
                TT("dve", m2, ps[:, 7, 0:MT], m2, ALU.subtract, [("ps", 7), "m2"], ["m2"])
                yield
                rsq(m2, m2, ["m2"], ["m2"])
                yield
                for j in range(4):
                    TT("pool", yc[:, j, :], yc[:, j, :], mean_sb, ALU.subtract, [yk(j), "mean_sb"], [yk(j)])
                    yield
                    TT("pool", yc[:, j, :], yc[:, j, :], m2, ALU.mult, [yk(j), "m2"], [yk(j)])
                    yield
                    ACT(yc[:, j, :], yc[:, j, :], AF.Identity, [yk(j), "pcols"], [yk(j)], scale=pc(l, 92 + j), bias=pc(l, 96 + j))
                    yield
                    ACT(sqc[j % 2], yc[:, j, :], AF.Tanh, [yk(j)], [("sqc", j % 2)], scale=0.5)
                    yield
                    STT("dve", yc[:, j, :], sqc[j % 2], 1.0, yc[:, j, :], ALU.add, ALU.mult, [yk(j), ("sqc", j % 2)], [yk(j)])
                    yield
                    ACT(sqB[j % 2], yc[:, j, :], AF.Square, [yk(j)], [("sqB", j % 2)])
                    MM(ps[:, 6, 0:MT], ones512[:], sqB[j % 2], j == 0, j == 3, [("sqB", j % 2), "ones512"], [("ps", 6)])
                    yield
                rsq(m2, ps[:, 6, 0:MT], [("ps", 6)], ["m2"], e4=True)
                yield
                for j in range(4):
                    TT("dve", yc[:, j, :], yc[:, j, :], m2, ALU.mult, [yk(j), "m2"], [yk(j)])
                    ACT(yb[:, 4 + j, :], yc[:, j, :], AF.Identity, [yk(j), "pcols"], [("yc", mt % 3, 4 + j, bi) for bi in range(NB)],
                        scale=pc(l, 100 + j))
                    yield
                ykeys = [[("yc", mt % 3, k, bi) for bi in range(NB)] for k in range(KC)]
                for dc in range(KC):
                    bo = fbank()
                    for k in range(KC):
                        MM(ps[:, bo, 0:MT], w_out[:, k, dc * 128:(dc + 1) * 128], yb[:, k, :], k == 0, k == KC - 1, ykeys[k] + ["w_out"], [("ps", bo)])
                    yield
                    ACT(yo[:, dc, :], ps[:, bo, 0:MT], AF.Copy, [("ps", bo)], [("yo", dc)])
                    ACT(sqF[dc % 2], ps[:, bo, 0:MT], AF.Square, [("ps", bo)], [("sqF", dc % 2)])
                    MM(ps[:, 7, 0:MT], onesD[:], sqF[dc % 2], dc == 0, dc == KC - 1, [("sqF", dc % 2), "onesD"], [("ps", 7)])
                    yield
                rsq(rsF, ps[:, 7, 0:MT], [("ps", 7)], ["rsF"])
                yield
                for dc in range(KC):
                    STT("dve", tmpF[dc % 2], yo[:, dc, :], dcols[:, l, 8 + dc:9 + dc], rsF, ALU.mult, ALU.mult,
                        [("yo", dc), "rsF", ("dc", l, 1)], [("tmpF", dc % 2)])
                    TT("pool", xT[:, dc, c0:c0 + MT], xT[:, dc, c0:c0 + MT], tmpF[dc % 2], ALU.add, [("x", dc, mt), ("tmpF", dc % 2)], [("x", dc, mt)])
                    yield

            def drive(gens):
                gens = [g for g in gens if g is not None]
                while gens:
                    for g in list(gens):
                        try:
                            next(g)
                        except StopIteration:
                            gens.remove(g)

            drive([front(0)])
            drive([conv(0), front(1)])
            for mt in range(1, NMT):
                drive([conv(mt), front(mt + 1) if mt + 1 < NMT else None, post(mt - 1)])
            drive([post(NMT - 1)])
            if do_mod_next:
                mod_fin(l + 1)
            S.barrier()

        def ffn(l, moe):
            A.reset()
            hT = A.alloc([KC, T], BF16)
            acc = A.alloc([KC, T], F32)
            acc_off = 32768
            wg = [A.alloc([KC, 256], BF16) for _ in range(2)]
            wu = [A.alloc([KC, 256], BF16) for _ in range(2)]
            wd = [A.alloc([2, D], BF16) for _ in range(2)]
            wslot_off = 32768 + 65536
            asl = [[A.alloc([FT], BF16) for _ in range(2)] for _ in range(2)]
            gate_bc = A.alloc([T], BF16)
            tsl = [A.alloc([FT], BF16) for _ in range(2)]
            dg = A.alloc([4, 128], F32)
            A2 = Arena(arena_t[:], acc_off + 65536)
            A2.off = acc_off
            sqA = [A2.alloc([FT], BF16) for _ in range(2)]
            rstdA = A2.alloc([FT], F32)
            tmpA = [A2.alloc([FT], F32) for _ in range(2)]
            h32b = [A2.alloc([FT], F32) for _ in range(2)]
            AG = Arena(arena_t[:], wslot_off + 24576)
            AG.off = wslot_off
            sqG = [AG.alloc([FT], BF16) for _ in range(2)]
            rsG = AG.alloc([FT], F32)
            tmpG = [AG.alloc([FT], F32) for _ in range(2)]

            if moe:
                DMA("sp", rw32[:], rw_d.rearrange("(k p) e -> p k e", p=128), [], ["rw32"], "rw32")
                DMA("sp", rb_bc[:], rb_d.partition_broadcast(128), [], ["rb_bc"], "rb_bc")
            for ft in range(NFT):
                c0 = ft * FT
                xk = lambda k, ft=ft: ("x", k, ft)

                def post(k, h32, hkey, ft=ft):
                    for b in range(4):
                        MM(ps[:, b, 0:8], h32[:, b * 128:(b + 1) * 128], rw32[:, k, :], k == 0, k == KC - 1, [hkey, "rw32"], [("ps", b)])

                norm_mod_tile(l, 2, c0, FT, sqA, rstdA, tmpA,
                              lambda k, ft=ft: (hT[:, k, ft * FT:(ft + 1) * FT], [("hT", k, ft)]),
                              xk, 6, h32_bufs=h32b if moe else None, post=post if moe else None)
                if moe:
                    for b in range(4):
                        TT("dve", lg[:, ft * 4 + b, :], ps[:, b, 0:8], rb_bc[:], ALU.add, [("ps", b), "rb_bc"], [("lg", ft * 4 + b)])
            if moe:
                lgk = [("lg", i) for i in range(16)]
                bc3 = lambda ap: ap.unsqueeze(2).to_broadcast([128, 16, NE])
                S.add("dve", lambda e: e.tensor_reduce(out=rt[:, 0, :], in_=lg[:], axis=AX.X, op=ALU.max), lgk, ["m1"])
                TT("dve", eq1[:], lg[:], bc3(rt[:, 0, :]), ALU.is_equal, lgk + ["m1"], ["eq1"])
                STT("dve", lg2[:], eq1[:], -1e30, lg[:], ALU.mult, ALU.add, ["eq1"] + lgk, ["lg2"])
                S.add("dve", lambda e: e.tensor_reduce(out=rt[:, 1, :], in_=lg2[:], axis=AX.X, op=ALU.max), ["lg2"], ["m2r"])
                TT("dve", eq2[:], lg2[:], bc3(rt[:, 1, :]), ALU.is_equal, ["lg2", "m2r"], ["eq2"])
                TT("dve", rt[:, 2, :], rt[:, 1, :], rt[:, 0, :], ALU.subtract, ["m1", "m2r"], ["dm"])
                ACT(rt[:, 3, :], rt[:, 2, :], AF.Sigmoid, ["dm"], ["w1"], scale=-1.0)
                ACT(rt[:, 4, :], rt[:, 2, :], AF.Sigmoid, ["dm"], ["w2"])
                TT("dve", eq1[:], eq1[:], bc3(rt[:, 3, :]), ALU.mult, ["eq1", "w1"], ["eq1"])
                TT("dve", eq2[:], eq2[:], bc3(rt[:, 4, :]), ALU.mult, ["eq2", "w2"], ["eq2"])
                TT("dve", gates[:], eq1[:], eq2[:], ALU.add, ["eq1", "eq2"], ["gates"])
            S.barrier()

            experts = list(range(NE)) if moe else [0]
            wgd = moe_wg_d if moe else ffn_wg_d
            wud = moe_wu_d if moe else ffn_wu_d
            wdd = moe_wd_d if moe else ffn_wd_d
            steps = []
            gi = 0
            for e_ in experts:
                for g in range(NFC // 2):
                    for ft in range(NFT):
                        steps.append((e_, g, ft, gi))
                    gi += 1
            dbank = [0]
            first_acc = {}

            def down(step):
                e_, g, ft, gidx = step
                s = gidx % 2
                aslot = asl[step_index[step] % 2]
                for dc in range(KC):
                    b = 4 + dbank[0] % 3
                    dbank[0] += 1
                    MM(ps[:, b, :], wd[s][:, 0, dc * 128:(dc + 1) * 128], aslot[0], True, False, [("wd", s), ("a", step_index[step] % 2, 0)], [("ps", b)])
                    MM(ps[:, b, :], wd[s][:, 1, dc * 128:(dc + 1) * 128], aslot[1], False, True, [("wd", s), ("a", step_index[step] % 2, 1)], [("ps", b)])
                    dst = acc[:, dc, ft * FT:(ft + 1) * FT]
                    if (dc, ft) not in first_acc:
                        first_acc[(dc, ft)] = True
                        ACT(dst, ps[:, b, :], AF.Copy, [("ps", b)], [("acc", dc, ft)])
                    else:
                        TT("dve", dst, dst, ps[:, b, :], ALU.add, [("ps", b), ("acc", dc, ft)], [("acc", dc, ft)])
                    yield

            def load_group(step):
                e2, g2, _, gidx2 = step
                s2 = gidx2 % 2
                f0 = 2 * g2 * 128
                DMA("pool", wg[s2], wgd[e2, :, f0:f0 + 256].rearrange("(k p) n -> p k n", p=128), [], [("wg", s2)], ("wg", s2))
                DMA("pool", wu[s2], wud[e2, :, f0:f0 + 256].rearrange("(k p) n -> p k n", p=128), [], [("wu", s2)], ("wu", s2))
                DMA("pool", wd[s2], wdd[e2, f0:f0 + 256, :].rearrange("(f p) n -> p f n", p=128), [], [("wd", s2)], ("wd", s2))

            step_index = {st_: i for i, st_ in enumerate(steps)}
            prev = None
            cur_e = None
            for si, step in enumerate(steps):
                e_, g, ft, gidx = step
                s = gidx % 2
                if si == 0:
                    load_group(step)
                if moe and e_ != cur_e:
                    cur_e = e_
                    for t4 in range(NFT):
                        for b in range(4):
                            TS("dve", dg[:, b, :], ident32[:], gates[:, t4 * 4 + b, e_:e_ + 1], None, ALU.mult, None, ["gates", "ident32"], [("dg", b)])
                        MM(ps[:, 7, :], ones32[:], dg.rearrange("p a b -> p (a b)"), True, True, [("dg", b) for b in range(4)] + ["ones32"], [("ps", 7)])
                        ACT(gate_bc[:, t4 * FT:(t4 + 1) * FT], ps[:, 7, :], AF.Copy, [("ps", 7)], [("gbc", t4)])
                ai = si % 2
                dgen = down(prev) if prev is not None else iter(())

                def dn(n):
                    for _ in range(n):
                        next(dgen, None)

                for f in range(2):
                    bg_, bu_ = 2 * f, 2 * f + 1
                    for k in range(KC):
                        MM(ps[:, bg_, :], wg[s][:, k, f * 128:(f + 1) * 128], hT[:, k, ft * FT:(ft + 1) * FT], k == 0, k == KC - 1,
                           [("wg", s), ("hT", k, ft)], [("ps", bg_)])
                    ACT(tsl[f], ps[:, bg_, :], AF.Silu, [("ps", bg_)], [("ts", f)])
                    if f == 1:
                        dn(2)
                    for k in range(KC):
                        MM(ps[:, bu_, :], wu[s][:, k, f * 128:(f + 1) * 128], hT[:, k, ft * FT:(ft + 1) * FT], k == 0, k == KC - 1,
                           [("wu", s), ("hT", k, ft)], [("ps", bu_)])
                    if moe:
                        TT("dve", tsl[f], tsl[f], ps[:, bu_, :], ALU.mult, [("ts", f), ("ps", bu_)], [("ts", f)])
                        TT("pool", asl[ai][f], tsl[f], gate_bc[:, ft * FT:(ft + 1) * FT], ALU.mult, [("ts", f), ("gbc", ft)], [("a", ai, f)])
                    else:
                        TT("dve", asl[ai][f], tsl[f], ps[:, bu_, :], ALU.mult, [("ts", f), ("ps", bu_)], [("a", ai, f)])
                    dn(2)
                dn(8)
                prev = step
                if ft == 0 and si + NFT < len(steps):
                    load_group(steps[si + NFT])
            for _ in down(prev):
                pass
            S.barrier()
            for ft in range(NFT):
                c0 = ft * FT
                for dc in range(KC):
                    ACT(sqG[dc % 2], acc[:, dc, c0:c0 + FT], AF.Square, [("acc", dc, ft)], [("sqG", dc % 2)])
                    MM(ps[:, 6, :], onesD[:], sqG[dc % 2], dc == 0, dc == KC - 1, [("sqG", dc % 2), "onesD"], [("ps", 6)])
                rsqrt_tile(rsG, ps[:, 6, :], [("ps", 6)], ["rsG"], "rsG_t")
                for dc in range(KC):
                    STT("dve", tmpG[dc % 2], acc[:, dc, c0:c0 + FT], dcols[:, l, 24 + dc:25 + dc], rsG, ALU.mult, ALU.mult,
                        [("acc", dc, ft), "rsG", ("dc", l, 3)], [("tmpG", dc % 2)])
                    TT("pool", xT[:, dc, c0:c0 + FT], xT[:, dc, c0:c0 + FT], tmpG[dc % 2], ALU.add, [("x", dc, ft), ("tmpG", dc % 2)], [("x", dc, ft)])
            S.barrier()

        A.reset()
        stage0 = [A.alloc([KC, 256], BF16) for _ in range(3)]
        for pi in range(24):
            mod_dma(0, pi, stage0)
            mod_mm(0, pi, stage0)
        mod_fin(0)
        S.barrier()
        done = False
        for l in range(n_layers):
            mixer(l, do_mod_next=(l + 1 < n_layers))
            if stop_after == ("mixer", l):
                break
            ffn(l, moe=(l % 2 == 1))
            if stop_after == ("ffn", l):
                break
        outs = []
        for k in range(KC):
            outs.append(DMA("sp", out_d[k * 128:(k + 1) * 128, :], xT[:, k, :], [("x", k, i) for i in range(NMT)] + [("x", k, i) for i in range(NFT)], [], ("out", k)))
        S.emit(nc, final_wait_ops=outs)
    return nc


def prep_inputs(inp):
    f = lambda a: np.ascontiguousarray(np.asarray(a, dtype=np.float32))
    x = f(inp["x"])
    c = f(inp["c"])
    fm = lambda v: np.ascontiguousarray(v.reshape(-1, 128).T)
    pcols = np.zeros((128, 2 * NPL), np.float32)
    b_ada, ng, b_in = f(inp["b_ada"]), f(inp["norm_gain"]), f(inp["b_in"])
    conv_b, lcg, lcb, gg = f(inp["conv_b"]), f(inp["ln_conv_gain"]), f(inp["ln_conv_bias"]), f(inp["group_gain"])
    conv_w, b_sp = f(inp["conv_w"]), f(inp["b_spatial"])
    for l in range(2):
        o = l * NPL
        pcols[:, o:o + 48] = fm(b_ada[l])
        for i in range(4):
            pcols[:, o + 48 + 8 * i:o + 56 + 8 * i] = fm(ng[l, i])
        pcols[:, o + 80:o + 88] = fm(b_in[l, 1024:])
        pcols[:, o + 88:o + 92] = fm(conv_b[l])
        pcols[:, o + 92:o + 96] = fm(lcg[l])
        pcols[:, o + 96:o + 100] = fm(lcb[l])
        pcols[:, o + 100:o + 104] = fm(gg[l, 512:])
        cw = conv_w[l].reshape(31, 4, 128)
        pcols[:, o + 104:o + 228] = cw.transpose(2, 0, 1).reshape(128, 124)
        pcols[:, o + 228:o + 236] = b_sp[l].T
    rowvecs = np.stack([np.stack([f(inp["ln_v_gain"])[l], f(inp["ln_v_bias"])[l], gg[l, :512]]) for l in range(2)])
    wsT = np.ascontiguousarray(f(inp["w_spatial"]).transpose(0, 3, 1, 2))
    shared = {
        "pcols": pcols,
        "ident": np.eye(128, dtype=np.float32),
        "w_ada": f(inp["w_ada"]),
        "w_in": f(inp["w_in"]),
        "w_out": f(inp["w_out"]),
        "b_in_row": np.ascontiguousarray(b_in[:, :1024]),
        "rowvecs": np.ascontiguousarray(rowvecs),
        "wsT": wsT,
        "ffn_wg": f(inp["ffn_w_gate"]),
        "ffn_wu": f(inp["ffn_w_up"]),
        "ffn_wd": f(inp["ffn_w_down"]),
        "router_w": f(inp["router_w"])[0],
        "router_b": f(inp["router_b"])[0],
        "moe_wg": f(inp["moe_w_gate"])[0],
        "moe_wu": f(inp["moe_w_up"])[0],
        "moe_wd": f(inp["moe_w_down"])[0],
    }
    maps = []
    for b in range(8):
        m = dict(shared)
        m["xT"] = np.ascontiguousarray(x[b].T)
        m["cT"] = fm(c[b])
        maps.append(m)
    return maps


_NC_CACHE = {}


def kernel(**inputs):
    maps = prep_inputs(inputs)
    if "nc" not in _NC_CACHE:
        _NC_CACHE["nc"] = build_program()
    res = run_bass_kernel_spmd(_NC_CACHE["nc"], maps, core_ids=list(range(8)))
    out = np.stack([np.ascontiguousarray(r["outT"].T) for r in res.results]).astype(np.float32)
    return out
```

```python
from contextlib import ExitStack

import numpy as np
import concourse.bass as bass
import concourse.mybir as mybir
from concourse.bass_utils import run_bass_kernel_spmd

F32 = mybir.dt.float32
BF16 = mybir.dt.bfloat16
AF = mybir.ActivationFunctionType
ALU = mybir.AluOpType
AX = mybir.AxisListType

D = 1024
T = 2048
KC = 8
DFF = 3584
NFC = 28
NE = 8
EPS = 1e-6
MT = 256
NMT = T // MT
FT = 512
NFT = T // FT
NPL = 236
COMPUTE = ("pe", "act", "dve", "pool")


class Op:
    __slots__ = ("eng", "fn", "deps", "sig", "val", "semkey", "nosync", "gi")


class Sched:
    def __init__(self):
        self.ops = {e: [] for e in ("pe", "act", "dve", "pool", "sp")}
        self.lastw = {}
        self.readers = {}
        self.n = 0
        self.dmas = []

    def add(self, eng, fn, reads=(), writes=(), dma=None, nosync=False):
        op = Op()
        op.eng = eng
        op.fn = fn
        op.sig = False
        op.val = None
        op.semkey = dma
        op.nosync = nosync
        op.gi = self.n
        deps = {}
        for k in reads:
            w = self.lastw.get(k)
            if w is not None:
                deps[id(w)] = w
        for k in writes:
            w = self.lastw.get(k)
            if w is not None:
                deps[id(w)] = w
            rd = self.readers.get(k)
            if rd:
                for r in rd.values():
                    deps[id(r)] = r
        op.deps = list(deps.values())
        for k in writes:
            self.lastw[k] = op
            self.readers[k] = {}
        for k in reads:
            rk = eng if dma is None else ("dma", self.n)
            self.readers.setdefault(k, {})[rk] = op
        self.ops[eng].append(op)
        if dma is not None:
            self.dmas.append(op)
        self.n += 1
        return op

    def barrier(self):
        lasts = []
        for lst in self.ops.values():
            for op in reversed(lst):
                if op.semkey is None and op.fn is not None:
                    lasts.append(op)
                    break
        deps = lasts + self.dmas
        for e in self.ops:
            op = Op()
            op.eng = e
            op.fn = None
            op.sig = False
            op.val = None
            op.semkey = None
            op.nosync = False
            op.gi = self.n
            op.deps = [d for d in deps]
            self.ops[e].append(op)
        self.dmas = []
        self.lastw = {}
        self.readers = {}

    def emit(self, nc, final_wait_ops=()):
        for e, lst in self.ops.items():
            for op in lst:
                for d in op.deps:
                    if d.semkey is None:
                        if d.eng == op.eng and op.semkey is None and op.nosync:
                            continue
                        d.sig = True
        cnt = {e: 0 for e in COMPUTE}
        dcnt = {}
        for e, lst in self.ops.items():
            for op in lst:
                if op.semkey is not None:
                    dcnt[op.semkey] = dcnt.get(op.semkey, 0) + 16
                    op.val = dcnt[op.semkey]
                elif op.sig:
                    cnt[op.eng] += 1
                    op.val = cnt[op.eng]
        with ExitStack() as st:
            sems = {e: st.enter_context(nc.semaphore("s_" + e)) for e in COMPUTE}
            dsems = {k: st.enter_context(nc.semaphore("d_" + str(i))) for i, k in enumerate(dcnt)}
            block = st.enter_context(nc.Block())
            sched = self

            def needs_of(op):
                need = {}
                for d in op.deps:
                    if d.semkey is not None:
                        s = dsems[d.semkey]
                    else:
                        if not d.sig:
                            continue
                        if d.eng == op.eng and op.semkey is None and op.nosync:
                            continue
                        s = sems[d.eng]
                    cur = need.get(s.num)
                    if cur is None or cur[1] < d.val:
                        need[s.num] = (s, d.val, d.gi)
                return need

            def run(eng_name, eng):
                known = {}
                lst = sched.ops[eng_name]
                needs = [needs_of(op) for op in lst]
                look = 6 if eng_name == "pe" else 0
                for j, op in enumerate(lst):
                    for num, (s, v, _) in needs[j].items():
                        if known.get(num, 0) < v:
                            vv = v
                            for jj in range(j + 1, min(len(lst), j + 1 + look)):
                                nx = needs[jj].get(num)
                                if nx is not None and nx[2] < op.gi and nx[1] > vv:
                                    vv = nx[1]
                            eng.wait_ge(s, vv)
                            known[num] = vv
                    if op.fn is None:
                        continue
                    ins = op.fn(eng)
                    if op.semkey is not None:
                        ins.then_inc(dsems[op.semkey], 16)
                    elif op.sig:
                        ins.then_inc(sems[op.eng], 1)
                if eng_name == "sp":
                    for op in final_wait_ops:
                        eng.wait_ge(dsems[op.semkey], op.val)

            block.sync(lambda e: run("sp", e))
            block.scalar(lambda e: run("act", e))
            block.vector(lambda e: run("dve", e))
            block.gpsimd(lambda e: run("pool", e))
            block.tensor(lambda e: run("pe", e))


class Arena:
    def __init__(self, ap, nbytes):
        self.ap = ap
        self.nbytes = nbytes
        self.off = 0

    def reset(self):
        self.off = 0

    def alloc(self, free_shape, dtype):
        esz = 4 if dtype == F32 else 2
        n = int(np.prod(free_shape)) * esz
        n_al = (n + 31) // 32 * 32
        assert self.off + n_al <= self.nbytes, ("arena overflow", self.off, n_al, self.nbytes)
        v = self.ap[:, self.off // 2:(self.off + n) // 2]
        self.off += n_al
        if dtype == F32:
            v = v.bitcast(F32)
        if len(free_shape) == 2:
            v = v.rearrange("p (a b) -> p a b", b=free_shape[1])
        elif len(free_shape) == 3:
            v = v.rearrange("p (a b c) -> p a b c", b=free_shape[1], c=free_shape[2])
        return v


def build_program(n_layers=2, stop_after=None):
    nc = bass.Bass("TRN2", target_bir_lowering=False)

    def din(name, shape):
        return nc.dram_tensor(name, list(shape), F32, kind="ExternalInput").ap()

    xT_d = din("xT", [D, T])
    cT_d = din("cT", [128, KC])
    pcols_d = din("pcols", [128, 2 * NPL])
    ident_d = din("ident", [128, 128])
    w_ada_d = din("w_ada", [2, D, 6 * D])
    w_in_d = din("w_in", [2, D, 2 * D])
    w_out_d = din("w_out", [2, D, D])
    b_in_row_d = din("b_in_row", [2, D])
    rowvecs_d = din("rowvecs", [2, 3, 512])
    wsT_d = din("wsT", [2, 128, 8, 128])
    ffn_wg_d = din("ffn_wg", [1, D, DFF])
    ffn_wu_d = din("ffn_wu", [1, D, DFF])
    ffn_wd_d = din("ffn_wd", [1, DFF, D])
    rw_d = din("router_w", [D, NE])
    rb_d = din("router_b", [NE])
    moe_wg_d = din("moe_wg", [NE, D, DFF])
    moe_wu_d = din("moe_wu", [NE, D, DFF])
    moe_wd_d = din("moe_wd", [NE, DFF, D])
    out_d = nc.dram_tensor("outT", [D, T], F32, kind="ExternalOutput").ap()

    ARENA_BYTES = 132 * 1024
    with ExitStack() as st:
        def sb(name, shape, dt):
            return st.enter_context(nc.sbuf_tensor(name, list(shape), dt))

        xT = sb("xT_sb", [128, KC, T], F32)
        pcols = sb("pcols_sb", [128, 2 * NPL], F32)
        modt = sb("modt", [128, 2, 48], F32)
        dcols = sb("dcols", [128, 2, 32], F32)
        ident32 = sb("ident32", [128, 128], F32)
        identbf = sb("identbf", [128, 128], BF16)
        onesD = sb("onesD", [128, 128], BF16)
        ones512 = sb("ones512", [128, 128], BF16)
        ones32_512 = sb("ones32_512", [128, 128], F32)
        ones32 = sb("ones32", [128, 128], F32)
        onesrow = sb("onesrow", [1, 128], BF16)
        epsc = sb("epsc", [128, 1], F32)
        cwh = sb("cwh", [128, 124], F32)
        eps4 = sb("eps4", [128, 1], F32)
        cst = sb("cst", [128, KC], F32)
        cact = sb("cact", [128, KC], BF16)
        small = sb("small", [128, 64], F32)
        lg = sb("lg", [128, 16, NE], F32)
        lg2 = sb("lg2", [128, 16, NE], F32)
        eq1 = sb("eq1", [128, 16, NE], F32)
        eq2 = sb("eq2", [128, 16, NE], F32)
        gates = sb("gates", [128, 16, NE], F32)
        rt = sb("rt", [128, 6, 16], F32)
        rw32 = sb("rw32", [128, KC, NE], F32)
        rb_bc = sb("rb_bc", [128, NE], F32)
        arena_t = sb("arena", [128, ARENA_BYTES // 2], BF16)
        ps = st.enter_context(nc.psum_tensor("ps", [128, 8, 512], F32))
        A = Arena(arena_t[:], ARENA_BYTES)
        S = Sched()

        def ACT(out, in_, func, r, w, bias=None, scale=None, accum=None, nosync=False):
            kw = {}
            if bias is not None:
                kw["bias"] = bias
            if scale is not None:
                kw["scale"] = scale
            if accum is not None:
                kw["accum_out"] = accum
            return S.add("act", lambda e: e.activation(out=out, in_=in_, func=func, **kw), r, w, nosync=nosync)

        def TT(eng, out, in0, in1, op, r, w, nosync=False):
            return S.add(eng, lambda e: e.tensor_tensor(out=out, in0=in0, in1=in1, op=op), r, w, nosync=nosync)

        def STT(eng, out, in0, scalar, in1, op0, op1, r, w, nosync=False):
            return S.add(eng, lambda e: e.scalar_tensor_tensor(out=out, in0=in0, scalar=scalar, in1=in1, op0=op0, op1=op1), r, w, nosync=nosync)

        def TS(eng, out, in0, s1, s2, op0, op1, r, w, nosync=False):
            if s2 is None:
                return S.add(eng, lambda e: e.tensor_scalar(out=out, in0=in0, scalar1=s1, scalar2=None, op0=op0), r, w, nosync=nosync)
            return S.add(eng, lambda e: e.tensor_scalar(out=out, in0=in0, scalar1=s1, scalar2=s2, op0=op0, op1=op1), r, w, nosync=nosync)

        def MM(out, lhsT, rhs, start, stop, r, w):
            return S.add("pe", lambda e: e.matmul(out, lhsT, rhs, start=start, stop=stop), r, w, nosync=True)

        def DMA(eng, out, in_, r, w, key):
            return S.add(eng, lambda e: e.dma_start(out=out, in_=in_), r, w, dma=key)

        def MEMSET(eng, ap, val, w):
            return S.add(eng, lambda e: e.memset(ap, val), (), w)

        def rsqrt_tile(out_sb, in_ps, r, w, wkey_tmp):
            ACT(out_sb, in_ps, AF.Sqrt, r + ["epsc"], w, bias=epsc[:, 0:1])
            S.add("dve", lambda e: e.reciprocal(out=out_sb, in_=out_sb), w, w)

        DMA("sp", pcols[:], pcols_d, [], ["pcols"], "pcols")
        DMA("sp", cst[:], cT_d, [], ["cst"], "cst")
        DMA("sp", ident32[:], ident_d, [], ["ident32"], "ident")
        for k in range(KC):
            DMA("sp", xT[:, k, :], xT_d[k * 128:(k + 1) * 128, :], [], [("x", k, mt) for mt in range(NMT)], ("xin", k))
        MEMSET("dve", onesD[:], 1.0 / D, ["onesD"])
        MEMSET("dve", ones512[:], 1.0 / 512, ["ones512"])
        MEMSET("dve", ones32_512[:], 1.0 / 512, ["ones32_512"])
        MEMSET("dve", ones32[:], 1.0, ["ones32"])
        MEMSET("dve", onesrow[:], 1.0, ["onesrow"])
        MEMSET("dve", epsc[:], EPS, ["epsc"])
        MEMSET("dve", eps4[:], 4.0 * EPS, ["eps4"])
        ACT(identbf[:], ident32[:], AF.Copy, ["ident32"], ["identbf"])
        ACT(cact[:], cst[:], AF.Silu, ["cst"], ["cact"])

        def pc(l, c0, n=1):
            return pcols[:, l * NPL + c0:l * NPL + c0 + n]

        def mod_dma(l, pi, stage):
            ns = len(stage)
            sl = stage[pi % ns]
            DMA("pool", sl, w_ada_d[l, :, pi * 256:(pi + 1) * 256].rearrange("(k p) n -> p k n", p=128), [], [("wada", pi % ns)], ("wada", pi % ns))

        def mod_mm(l, pi, stage):
            ns = len(stage)
            sl = stage[pi % ns]
            for o in range(2):
                oc = pi * 2 + o
                for k in range(KC):
                    MM(ps[:, 5, 256 + oc:256 + oc + 1], sl[:, k, o * 128:(o + 1) * 128], cact[:, k:k + 1], k == 0, k == KC - 1,
                       [("wada", pi % ns), "cact"], ["psmod"])

        def mod_fin(l):
            TT("dve", modt[:, l, :], ps[:, 5, 256:304], pc(l, 0, 48), ALU.add, ["psmod", "pcols"], [("mod", l)])
            STT("dve", dcols[:, l, 0:8], modt[:, l, 8:16], 1.0, pc(l, 48, 8), ALU.add, ALU.mult, [("mod", l), "pcols"], [("dc", l, 0)])
            TT("dve", dcols[:, l, 8:16], modt[:, l, 16:24], pc(l, 56, 8), ALU.mult, [("mod", l), "pcols"], [("dc", l, 1)])
            STT("dve", dcols[:, l, 16:24], modt[:, l, 32:40], 1.0, pc(l, 64, 8), ALU.add, ALU.mult, [("mod", l), "pcols"], [("dc", l, 2)])
            TT("dve", dcols[:, l, 24:32], modt[:, l, 40:48], pc(l, 72, 8), ALU.mult, [("mod", l), "pcols"], [("dc", l, 3)])

        def norm_mod_tile(l, which, c0, ncol, sq_bufs, rstd_buf, tmp_bufs, out_fn, xkeys_fn, stat_bank, h32_bufs=None, post=None):
            gsc = 0 if which == 1 else 16
            shc = 0 if which == 1 else 24
            for k in range(KC):
                sq = sq_bufs[k % 2]
                ACT(sq, xT[:, k, c0:c0 + ncol], AF.Square, [xkeys_fn(k)], [("sqA", k % 2)])
                MM(ps[:, stat_bank, 0:ncol], onesD[:], sq, k == 0, k == KC - 1, [("sqA", k % 2), "onesD"], [("ps", stat_bank)])
            rsqrt_tile(rstd_buf, ps[:, stat_bank, 0:ncol], [("ps", stat_bank)], ["rstdA"], "rstdA_t")
            for k in range(KC):
                tmp = tmp_bufs[k % 2]
                TT("dve", tmp, xT[:, k, c0:c0 + ncol], rstd_buf, ALU.mult, [xkeys_fn(k), "rstdA"], [("tmpA", k % 2)])
                dst, dkeys = out_fn(k)
                if h32_bufs is None:
                    ACT(dst, tmp, AF.Identity, [("tmpA", k % 2), ("dc", l, 0 if which == 1 else 2), ("mod", l)], dkeys,
                        scale=dcols[:, l, gsc + k:gsc + k + 1], bias=modt[:, l, shc + k:shc + k + 1])
                else:
                    h32 = h32_bufs[k % 2]
                    ACT(h32, tmp, AF.Identity, [("tmpA", k % 2), ("dc", l, 2), ("mod", l)], [("h32", k % 2)],
                        scale=dcols[:, l, gsc + k:gsc + k + 1], bias=modt[:, l, shc + k:shc + k + 1])
                    S.add("pool", lambda e, dst=dst, h32=h32: e.tensor_copy(out=dst, in_=h32), [("h32", k % 2)], dkeys)
                    post(k, h32, ("h32", k % 2))

        def mixer(l, do_mod_next):
            A.reset()
            w_in = A.alloc([KC, 2 * D], BF16)
            w_out = A.alloc([KC, D], BF16)
            hb = A.alloc([KC, MT], BF16)
            ycat = [A.alloc([KC, MT], BF16) for _ in range(3)]
            wsT = A.alloc([8, 128], BF16)
            bct = A.alloc([3, 512], F32)
            binrow = A.alloc([D], BF16)
            ub = [A.alloc([512], F32) for _ in range(2)]
            gv = A.alloc([512], F32)
            vb = [A.alloc([512], BF16) for _ in range(2)]
            yanb = [A.alloc([512], BF16) for _ in range(2)]
            junk = A.alloc([512], BF16)
            sg = A.alloc([MT], F32)
            abb = junk.bitcast(F32)
            xg = [A.alloc([4, 30 + MT], F32) for _ in range(2)]
            ycb = [A.alloc([4, MT], F32) for _ in range(2)]
            sqc = [A.alloc([MT], F32) for _ in range(2)]
            mean_sb = A.alloc([MT], F32)
            m2 = A.alloc([MT], F32)
            sqA = [A.alloc([MT], BF16) for _ in range(2)]
            sqB = [A.alloc([MT], BF16) for _ in range(2)]
            rstdA = A.alloc([MT], F32)
            tmpA = [A.alloc([MT], F32) for _ in range(2)]
            yo = A.alloc([KC, MT], F32)
            sqF = [A.alloc([MT], BF16) for _ in range(2)]
            rsF = A.alloc([MT], F32)
            tmpF = [A.alloc([MT], F32) for _ in range(2)]
            stage = [A.alloc([KC, 256], BF16) for _ in range(2)]

            DMA("pool", w_in, w_in_d[l].rearrange("(k p) n -> p k n", p=128), [], ["w_in"], "w_in")
            DMA("pool", wsT, wsT_d[l], [], ["wsT_raw"], "wsT")
            DMA("pool", binrow[0:1, :], b_in_row_d[l:l + 1, :], [], ["binrow"], "binrow")
            DMA("sp", bct, rowvecs_d[l].partition_broadcast(128), [], ["bct"], "bct")
            DMA("pool", w_out, w_out_d[l].rearrange("(k p) n -> p k n", p=128), [], ["w_out"], "w_out")
            MEMSET("dve", wsT[64:128, :, 0:64], 0.0, ["wsT_raw"])
            MEMSET("dve", xg[0][:, :, 0:30], 0.0, [("xg_halo", 0)])
            TS("dve", small[:, 32:36], pc(l, 84, 4), 0.5, None, ALU.mult, None, ["pcols"], ["hbcol"])
            TS("dve", cwh[:], pc(l, 104, 124), 0.5, None, ALU.mult, None, ["pcols"], ["cwh"])
            zi = [0]
            fi = [0]

            def zbank():
                zi[0] += 1
                return zi[0] % 2

            def fbank():
                fi[0] += 1
                return 2 + fi[0] % 2

            NB = MT // 128

            def rsq(out_sb, in_ap, r, w, e4=False):
                ACT(out_sb, in_ap, AF.Sqrt, r + ["epsc", "eps4"], w, bias=(eps4 if e4 else epsc)[:, 0:1])
                S.add("dve", lambda e: e.reciprocal(out=out_sb, in_=out_sb), w, w)

            def front(mt):
                c0 = mt * MT
                yb = ycat[mt % 3]
                xgb = xg[mt % 2]
                hk = lambda k: ("hT", k)
                if do_mod_next:
                    mod_dma(l + 1, 3 * mt, stage)
                    mod_dma(l + 1, 3 * mt + 1, stage)
                for k in range(KC):
                    sq = sqA[k % 2]
                    ACT(sq, xT[:, k, c0:c0 + MT], AF.Square, [("x", k, mt)], [("sqA", k % 2)])
                    MM(ps[:, 4, 0:MT], onesD[:], sq, k == 0, k == KC - 1, [("sqA", k % 2), "onesD"], [("ps", 4)])
                    yield
                rsq(rstdA, ps[:, 4, 0:MT], [("ps", 4)], ["rstdA"])
                yield
                for k in range(KC):
                    tmp = tmpA[k % 2]
                    TT("dve", tmp, xT[:, k, c0:c0 + MT], rstdA, ALU.mult, [("x", k, mt), "rstdA"], [("tmpA", k % 2)])
                    ACT(hb[:, k, :], tmp, AF.Identity, [("tmpA", k % 2), ("dc", l, 0), ("mod", l)], [hk(k)],
                        scale=dcols[:, l, k:k + 1], bias=modt[:, l, k:k + 1])
                    yield
                for bi in range(NB):
                    tb = mt * NB + bi
                    b0 = bi * 128
                    pb = tb % 2
                    u = ub[pb]
                    v = vb[pb]
                    yan = yanb[pb]
                    bu = zbank()
                    for k in range(KC):
                        MM(ps[:, bu, :], hb[:, k, b0:b0 + 128], w_in[:, k, 0:512], k == 0, False, [hk(k), "w_in"], [("ps", bu)])
                    MM(ps[:, bu, :], onesrow[0:1, :], binrow[0:1, 0:512], False, True, ["onesrow", "binrow"], [("ps", bu)])
                    yield
                    bv = zbank()
                    for k in range(KC):
                        MM(ps[:, bv, :], hb[:, k, b0:b0 + 128], w_in[:, k, 512:1024], k == 0, False, [hk(k), "w_in"], [("ps", bv)])
                    MM(ps[:, bv, :], onesrow[0:1, :], binrow[0:1, 512:1024], False, True, ["onesrow", "binrow"], [("ps", bv)])
                    yield
                    ACT(u, ps[:, bu, :], AF.Gelu_apprx_tanh, [("ps", bu)], [("u", pb)])
                    yield
                    ACT(gv, ps[:, bv, :], AF.Gelu_apprx_tanh, [("ps", bv)], ["gv"])
                    yield
                    s0 = pb * 16
                    S.add("dve", lambda e, s0=s0: e.bn_stats(out=small[:, s0:s0 + 6], in_=gv), ["gv"], [("st6", pb)])
                    S.add("dve", lambda e, s0=s0: e.bn_aggr(out=small[:, s0 + 6:s0 + 8], in_=small[:, s0:s0 + 6]), [("st6", pb)], [("mv", pb)])
                    yield
                    ACT(small[:, s0 + 8:s0 + 9], small[:, s0 + 7:s0 + 8], AF.Sqrt, [("mv", pb), "epsc"], [("sd", pb)], bias=epsc[:, 0:1])
                    S.add("dve", lambda e, s0=s0: e.reciprocal(out=small[:, s0 + 9:s0 + 10], in_=small[:, s0 + 8:s0 + 9]), [("sd", pb)], [("rv", pb)])
                    yield
                    STT("dve", small[:, s0 + 10:s0 + 11], small[:, s0 + 6:s0 + 7], -1.0, small[:, s0 + 9:s0 + 10], ALU.mult, ALU.mult,
                        [("mv", pb), ("rv", pb)], [("nb", pb)])
                    yield
                    ACT(gv, gv, AF.Identity, ["gv", ("rv", pb), ("nb", pb)], ["gv"],
                        scale=small[:, s0 + 9:s0 + 10], bias=small[:, s0 + 10:s0 + 11])
                    yield
                    TT("dve", gv, gv, bct[:, 0, :], ALU.mult, ["gv", "bct"], ["gv"])
                    yield
                    TT("dve", v, gv, bct[:, 1, :], ALU.add, ["gv", "bct"], [("v", pb)])
                    yield
                    bm = 4
                    for h in range(8):
                        MM(ps[:, bm, h * 64:(h + 1) * 64], wsT[:, h, :], v[:, h * 64:(h + 1) * 64], True, True,
                           ["wsT_raw", ("v", pb)], [("ps", bm)])
                    yield
                    for h in range(8):
                        STT("dve", u[:, h * 64:(h + 1) * 64], ps[:, bm, h * 64:(h + 1) * 64], pc(l, 228 + h), u[:, h * 64:(h + 1) * 64],
                            ALU.add, ALU.mult, [("ps", bm), ("u", pb), "pcols"], [("u", pb)], nosync=(h > 0))
                        if h % 2 == 1:
                            yield
                    MEMSET("dve", small[:, s0 + 11:s0 + 12], 0.0, [("ssA", pb)])
                    ACT(junk, u, AF.Square, [("u", pb), ("ssA", pb)], ["junk", ("ssA", pb)], accum=small[:, s0 + 11:s0 + 12])
                    yield
                    TS("dve", small[:, s0 + 12:s0 + 13], small[:, s0 + 11:s0 + 12], 1.0 / 512, EPS, ALU.mult, ALU.add, [("ssA", pb)], [("ssA2", pb)])
                    ACT(small[:, s0 + 13:s0 + 14], small[:, s0 + 12:s0 + 13], AF.Sqrt, [("ssA2", pb)], [("sdA", pb)])
                    S.add("dve", lambda e, s0=s0: e.reciprocal(out=small[:, s0 + 14:s0 + 15], in_=small[:, s0 + 13:s0 + 14]), [("sdA", pb)], [("rsA", pb)])
                    yield
                    STT("dve", yan, u, small[:, s0 + 14:s0 + 15], bct[:, 2, :], ALU.mult, ALU.mult, [("u", pb), ("rsA", pb), "bct"], [("yan", pb)])
                    yield
                    psT = ps[:, 5, 0:256].bitcast(BF16).rearrange("p (j t) -> p j t", t=128)
                    for j in range(4):
                        S.add("pe", lambda e, j=j, yan=yan, psT=psT: e.transpose(out=psT[:, j, :], in_=yan[:, j * 128:(j + 1) * 128], identity=identbf[:]),
                              [("yan", pb), "identbf"], [("ps", 5)], nosync=True)
                    ACT(yb[:, 0:4, b0:b0 + 128], psT, AF.Copy, [("ps", 5)], [("yc", mt % 3, j, bi) for j in range(4)])
                    yield
                if mt > 0:
                    S.add("pool", lambda e, xgb=xgb, prev=xg[(mt - 1) % 2]: e.tensor_copy(out=xgb[:, :, 0:30], in_=prev[:, :, MT:MT + 30]),
                          [("xg", (mt - 1) % 2, j) for j in range(4)], [("xg_halo", mt % 2)])
                for j in range(4):
                    ba = zbank()
                    for k in range(KC):
                        MM(ps[:, ba, 0:MT], w_in[:, k, (8 + j) * 128:(9 + j) * 128], hb[:, k, :], k == 0, k == KC - 1, [hk(k), "w_in"], [("ps", ba)])
                    yield
                    bg = zbank()
                    for k in range(KC):
                        MM(ps[:, bg, 0:MT], w_in[:, k, (12 + j) * 128:(13 + j) * 128], hb[:, k, :], k == 0, k == KC - 1, [hk(k), "w_in"], [("ps", bg)])
                    yield
                    ACT(sg, ps[:, bg, 0:MT], AF.Tanh, [("ps", bg), "hbcol"], ["sg"], scale=0.5, bias=small[:, 32 + j:33 + j])
                    ACT(abb, ps[:, ba, 0:MT], AF.Identity, [("ps", ba), "pcols"], ["junk"], bias=pc(l, 80 + j))
                    yield
                    STT("dve", xgb[:, j, 30:30 + MT], sg, 1.0, abb, ALU.add, ALU.mult,
                        ["junk", "sg"], [("xg", mt % 2, j)])
                    yield
                if do_mod_next:
                    mod_mm(l + 1, 3 * mt, stage)
                    yield
                    mod_dma(l + 1, 3 * mt + 2, stage)
                    mod_mm(l + 1, 3 * mt + 1, stage)
                    yield
                    mod_mm(l + 1, 3 * mt + 2, stage)
                    yield

            def conv(mt):
                xgb = xg[mt % 2]
                yc = ycb[mt % 2]
                xr = lambda j: [("xg", mt % 2, j), ("xg_halo", mt % 2)]
                for j in range(4):
                    TS("dve", yc[:, j, :], xgb[:, j, 0:MT], cwh[:, j:j + 1], pc(l, 88 + j), ALU.mult, ALU.add,
                       xr(j) + ["pcols", "cwh"], [("ycv", mt % 2, j)])
                    yield
                    for tap in range(1, 31):
                        STT("dve", yc[:, j, :], xgb[:, j, tap:tap + MT], cwh[:, tap * 4 + j:tap * 4 + j + 1], yc[:, j, :], ALU.mult, ALU.add,
                            xr(j) + [("ycv", mt % 2, j)], [("ycv", mt % 2, j)], nosync=True)
                        yield

            def post(mt):
                c0 = mt * MT
                yb = ycat[mt % 3]
                yc = ycb[mt % 2]
                yk = lambda j: ("ycv", mt % 2, j)
                for j in range(4):
                    MM(ps[:, 6, 0:MT], ones32_512[:], yc[:, j, :], j == 0, j == 3, [yk(j), "ones32_512"], [("ps", 6)])
                yield
                for j in range(4):
                    ACT(sqc[j % 2], yc[:, j, :], AF.Square, [yk(j)], [("sqc", j % 2)])
                    MM(ps[:, 7, 0:MT], ones32_512[:], sqc[j % 2], j == 0, j == 3, [("sqc", j % 2), "ones32_512"], [("ps", 7)])
                    yield
                ACT(mean_sb, ps[:, 6, 0:MT], AF.Copy, [("ps", 6)], ["mean_sb"])
                yield
                TT("dve", m2, mean_sb, mean_sb, ALU.mult, ["mean_sb"], ["m2"])
                yield
                TT("dve", m2, ps[:, 7, 0:MT], m2, ALU.subtract, [("ps", 7), "m2"], ["m2"])
                yield
                rsq(m2, m2, ["m2"], ["m2"])
                yield
                for j in range(4):
                    TT("pool", yc[:, j, :], yc[:, j, :], mean_sb, ALU.subtract, [yk(j), "mean_sb"], [yk(j)])
                    yield
                    TT("pool", yc[:, j, :], yc[:, j, :], m2, ALU.mult, [yk(j), "m2"], [yk(j)])
                    yield
                    ACT(yc[:, j, :], yc[:, j, :], AF.Identity, [yk(j), "pcols"], [yk(j)], scale=pc(l, 92 + j), bias=pc(l, 96 + j))
                    yield
                    ACT(sqc[j % 2], yc[:, j, :], AF.Tanh, [yk(j)], [("sqc", j % 2)], scale=0.5)
                    yield
                    STT("dve", yc[:, j, :], sqc[j % 2], 1.0, yc[:, j, :], ALU.add, ALU.mult, [yk(j), ("sqc", j % 2)], [yk(j)])
                    yield
                    ACT(sqB[j % 2], yc[:, j, :], AF.Square, [yk(j)], [("sqB", j % 2)])
                    MM(ps[:, 6, 0:MT], ones512[:], sqB[j % 2], j == 0, j == 3, [("sqB", j % 2), "ones512"], [("ps", 6)])
                    yield
                rsq(m2, ps[:, 6, 0:MT], [("ps", 6)], ["m2"], e4=True)
                yield
                for j in range(4):
                    TT("dve", yc[:, j, :], yc[:, j, :], m2, ALU.mult, [yk(j), "m2"], [yk(j)])
                    ACT(yb[:, 4 + j, :], yc[:, j, :], AF.Identity, [yk(j), "pcols"], [("yc", mt % 3, 4 + j, bi) for bi in range(NB)],
                        scale=pc(l, 100 + j))
                    yield
                ykeys = [[("yc", mt % 3, k, bi) for bi in range(NB)] for k in range(KC)]
                for dc in range(KC):
                    bo = fbank()
                    for k in range(KC):
                        MM(ps[:, bo, 0:MT], w_out[:, k, dc * 128:(dc + 1) * 128], yb[:, k, :], k == 0, k == KC - 1, ykeys[k] + ["w_out"], [("ps", bo)])
                    yield
                    ACT(yo[:, dc, :], ps[:, bo, 0:MT], AF.Copy, [("ps", bo)], [("yo", dc)])
                    ACT(sqF[dc % 2], ps[:, bo, 0:MT], AF.Square, [("ps", bo)], [("sqF", dc % 2)])
                    MM(ps[:, 7, 0:MT], onesD[:], sqF[dc % 2], dc == 0, dc == KC - 1, [("sqF", dc % 2), "onesD"], [("ps", 7)])
                    yield
                rsq(rsF, ps[:, 7, 0:MT], [("ps", 7)], ["rsF"])
                yield
                for dc in range(KC):
                    STT("dve", tmpF[dc % 2], yo[:, dc, :], dcols[:, l, 8 + dc:9 + dc], rsF, ALU.mult, ALU.mult,
                        [("yo", dc), "rsF", ("dc", l, 1)], [("tmpF", dc % 2)])
                    TT("pool", xT[:, dc, c0:c0 + MT], xT[:, dc, c0:c0 + MT], tmpF[dc % 2], ALU.add, [("x", dc, mt), ("tmpF", dc % 2)], [("x", dc, mt)])
                    yield

            def drive(gens):
                gens = [g for g in gens if g is not None]
                while gens:
                    for g in list(gens):
                        try:
                            next(g)
                        except StopIteration:
                            gens.remove(g)

            drive([front(0)])
            drive([conv(0), front(1)])
            for mt in range(1, NMT):
                drive([conv(mt), front(mt + 1) if mt + 1 < NMT else None, post(mt - 1)])
            drive([post(NMT - 1)])
            if do_mod_next:
                mod_fin(l + 1)
            S.barrier()

        def ffn(l, moe):
            A.reset()
            hT = A.alloc([KC, T], BF16)
            acc = A.alloc([KC, T], F32)
            acc_off = 32768
            wg = [A.alloc([KC, 256], BF16) for _ in range(2)]
            wu = [A.alloc([KC, 256], BF16) for _ in range(2)]
            wd = [A.alloc([2, D], BF16) for _ in range(2)]
            wslot_off = 32768 + 65536
            asl = [[A.alloc([FT], BF16) for _ in range(2)] for _ in range(2)]
            gate_bc = A.alloc([T], BF16)
            tsl = [A.alloc([FT], BF16) for _ in range(2)]
            dg = A.alloc([4, 128], F32)
            A2 = Arena(arena_t[:], acc_off + 65536)
            A2.off = acc_off
            sqA = [A2.alloc([FT], BF16) for _ in range(2)]
            rstdA = A2.alloc([FT], F32)
            tmpA = [A2.alloc([FT], F32) for _ in range(2)]
            h32b = [A2.alloc([FT], F32) for _ in range(2)]
            AG = Arena(arena_t[:], wslot_off + 24576)
            AG.off = wslot_off
            sqG = [AG.alloc([FT], BF16) for _ in range(2)]
            rsG = AG.alloc([FT], F32)
            tmpG = [AG.alloc([FT], F32) for _ in range(2)]

            if moe:
                DMA("sp", rw32[:], rw_d.rearrange("(k p) e -> p k e", p=128), [], ["rw32"], "rw32")
                DMA("sp", rb_bc[:], rb_d.partition_broadcast(128), [], ["rb_bc"], "rb_bc")
            for ft in range(NFT):
                c0 = ft * FT
                xk = lambda k, ft=ft: ("x", k, ft)

                def post(k, h32, hkey, ft=ft):
                    for b in range(4):
                        MM(ps[:, b, 0:8], h32[:, b * 128:(b + 1) * 128], rw32[:, k, :], k == 0, k == KC - 1, [hkey, "rw32"], [("ps", b)])

                norm_mod_tile(l, 2, c0, FT, sqA, rstdA, tmpA,
                              lambda k, ft=ft: (hT[:, k, ft * FT:(ft + 1) * FT], [("hT", k, ft)]),
                              xk, 6, h32_bufs=h32b if moe else None, post=post if moe else None)
                if moe:
                    for b in range(4):
                        TT("dve", lg[:, ft * 4 + b, :], ps[:, b, 0:8], rb_bc[:], ALU.add, [("ps", b), "rb_bc"], [("lg", ft * 4 + b)])
            if moe:
                lgk = [("lg", i) for i in range(16)]
                bc3 = lambda ap: ap.unsqueeze(2).to_broadcast([128, 16, NE])
                S.add("dve", lambda e: e.tensor_reduce(out=rt[:, 0, :], in_=lg[:], axis=AX.X, op=ALU.max), lgk, ["m1"])
                TT("dve", eq1[:], lg[:], bc3(rt[:, 0, :]), ALU.is_equal, lgk + ["m1"], ["eq1"])
                STT("dve", lg2[:], eq1[:], -1e30, lg[:], ALU.mult, ALU.add, ["eq1"] + lgk, ["lg2"])
                S.add("dve", lambda e: e.tensor_reduce(out=rt[:, 1, :], in_=lg2[:], axis=AX.X, op=ALU.max), ["lg2"], ["m2r"])
                TT("dve", eq2[:], lg2[:], bc3(rt[:, 1, :]), ALU.is_equal, ["lg2", "m2r"], ["eq2"])
                TT("dve", rt[:, 2, :], rt[:, 1, :], rt[:, 0, :], ALU.subtract, ["m1", "m2r"], ["dm"])
                ACT(rt[:, 3, :], rt[:, 2, :], AF.Sigmoid, ["dm"], ["w1"], scale=-1.0)
                ACT(rt[:, 4, :], rt[:, 2, :], AF.Sigmoid, ["dm"], ["w2"])
                TT("dve", eq1[:], eq1[:], bc3(rt[:, 3, :]), ALU.mult, ["eq1", "w1"], ["eq1"])
                TT("dve", eq2[:], eq2[:], bc3(rt[:, 4, :]), ALU.mult, ["eq2", "w2"], ["eq2"])
                TT("dve", gates[:], eq1[:], eq2[:], ALU.add, ["eq1", "eq2"], ["gates"])
            S.barrier()

            experts = list(range(NE)) if moe else [0]
            wgd = moe_wg_d if moe else ffn_wg_d
            wud = moe_wu_d if moe else ffn_wu_d
            wdd = moe_wd_d if moe else ffn_wd_d
            steps = []
            gi = 0
            for e_ in experts:
                for g in range(NFC // 2):
                    for ft in range(NFT):
                        steps.append((e_, g, ft, gi))
                    gi += 1
            dbank = [0]
            first_acc = {}

            def down(step):
                e_, g, ft, gidx = step
                s = gidx % 2
                aslot = asl[step_index[step] % 2]
                for dc in range(KC):
                    b = 4 + dbank[0] % 4
                    dbank[0] += 1
                    MM(ps[:, b, :], wd[s][:, 0, dc * 128:(dc + 1) * 128], aslot[0], True, False, [("wd", s), ("a", step_index[step] % 2, 0)], [("ps", b)])
                    MM(ps[:, b, :], wd[s][:, 1, dc * 128:(dc + 1) * 128], aslot[1], False, True, [("wd", s), ("a", step_index[step] % 2, 1)], [("ps", b)])
                    dst = acc[:, dc, ft * FT:(ft + 1) * FT]
                    if (dc, ft) not in first_acc:
                        first_acc[(dc, ft)] = True
                        ACT(dst, ps[:, b, :], AF.Copy, [("ps", b)], [("acc", dc, ft)])
                    else:
                        TT("dve", dst, dst, ps[:, b, :], ALU.add, [("ps", b), ("acc", dc, ft)], [("acc", dc, ft)])
                    yield

            def load_group(step):
                e2, g2, _, gidx2 = step
                s2 = gidx2 % 2
                f0 = 2 * g2 * 128
                DMA("pool", wg[s2], wgd[e2, :, f0:f0 + 256].rearrange("(k p) n -> p k n", p=128), [], [("wg", s2)], ("wg", s2))
                DMA("pool", wu[s2], wud[e2, :, f0:f0 + 256].rearrange("(k p) n -> p k n", p=128), [], [("wu", s2)], ("wu", s2))
                DMA("pool", wd[s2], wdd[e2, f0:f0 + 256, :].rearrange("(f p) n -> p f n", p=128), [], [("wd", s2)], ("wd", s2))

            step_index = {st_: i for i, st_ in enumerate(steps)}
            prev = None
            cur_e = None
            for si, step in enumerate(steps):
                e_, g, ft, gidx = step
                s = gidx % 2
                if si == 0:
                    load_group(step)
                if moe and e_ != cur_e:
                    cur_e = e_
                    for t4 in range(NFT):
                        for b in range(4):
                            TS("dve", dg[:, b, :], ident32[:], gates[:, t4 * 4 + b, e_:e_ + 1], None, ALU.mult, None, ["gates", "ident32"], [("dg", b)])
                        MM(ps[:, 7, :], ones32[:], dg.rearrange("p a b -> p (a b)"), True, True, [("dg", b) for b in range(4)] + ["ones32"], [("ps", 7)])
                        ACT(gate_bc[:, t4 * FT:(t4 + 1) * FT], ps[:, 7, :], AF.Copy, [("ps", 7)], [("gbc", t4)])
                ai = si % 2
                dgen = down(prev) if prev is not None else iter(())

                def dn(n):
                    for _ in range(n):
                        next(dgen, None)

                for f in range(2):
                    bg_, bu_ = 2 * f, 2 * f + 1
                    for k in range(KC):
                        MM(ps[:, bg_, :], wg[s][:, k, f * 128:(f + 1) * 128], hT[:, k, ft * FT:(ft + 1) * FT], k == 0, k == KC - 1,
                           [("wg", s), ("hT", k, ft)], [("ps", bg_)])
                    ACT(tsl[f], ps[:, bg_, :], AF.Silu, [("ps", bg_)], [("ts", f)])
                    if f == 1:
                        dn(2)
                    for k in range(KC):
                        MM(ps[:, bu_, :], wu[s][:, k, f * 128:(f + 1) * 128], hT[:, k, ft * FT:(ft + 1) * FT], k == 0, k == KC - 1,
                           [("wu", s), ("hT", k, ft)], [("ps", bu_)])
                    if moe:
                        TT("dve", tsl[f], tsl[f], ps[:, bu_, :], ALU.mult, [("ts", f), ("ps", bu_)], [("ts", f)])
                        TT("pool", asl[ai][f], tsl[f], gate_bc[:, ft * FT:(ft + 1) * FT], ALU.mult, [("ts", f), ("gbc", ft)], [("a", ai, f)])
                    else:
                        TT("dve", asl[ai][f], tsl[f], ps[:, bu_, :], ALU.mult, [("ts", f), ("ps", bu_)], [("a", ai, f)])
                    dn(2)
                dn(8)
                prev = step
                if ft == 0 and si + NFT < len(steps):
                    load_group(steps[si + NFT])
            for _ in down(prev):
                pass
            S.barrier()
            for ft in range(NFT):
                c0 = ft * FT
                for dc in range(KC):
                    ACT(sqG[dc % 2], acc[:, dc, c0:c0 + FT], AF.Square, [("acc", dc, ft)], [("sqG", dc % 2)])
                    MM(ps[:, 6, :], onesD[:], sqG[dc % 2], dc == 0, dc == KC - 1, [("sqG", dc % 2), "onesD"], [("ps", 6)])
                rsqrt_tile(rsG, ps[:, 6, :], [("ps", 6)], ["rsG"], "rsG_t")
                for dc in range(KC):
                    STT("dve", tmpG[dc % 2], acc[:, dc, c0:c0 + FT], dcols[:, l, 24 + dc:25 + dc], rsG, ALU.mult, ALU.mult,
                        [("acc", dc, ft), "rsG", ("dc", l, 3)], [("tmpG", dc % 2)])
                    TT("pool", xT[:, dc, c0:c0 + FT], xT[:, dc, c0:c0 + FT], tmpG[dc % 2], ALU.add, [("x", dc, ft), ("tmpG", dc % 2)], [("x", dc, ft)])
            S.barrier()

        A.reset()
        stage0 = [A.alloc([KC, 256], BF16) for _ in range(3)]
        for pi in range(24):
            mod_dma(0, pi, stage0)
            mod_mm(0, pi, stage0)
        mod_fin(0)
        S.barrier()
        done = False
        for l in range(n_layers):
            mixer(l, do_mod_next=(l + 1 < n_layers))
            if stop_after == ("mixer", l):
                break
            ffn(l, moe=(l % 2 == 1))
            if stop_after == ("ffn", l):
                break
        outs = []
        for k in range(KC):
            outs.append(DMA("sp", out_d[k * 128:(k + 1) * 128, :], xT[:, k, :], [("x", k, i) for i in range(NMT)] + [("x", k, i) for i in range(NFT)], [], ("out", k)))
        S.emit(nc, final_wait_ops=outs)
    return nc


def prep_inputs(inp):
    f = lambda a: np.ascontiguousarray(np.asarray(a, dtype=np.float32))
    x = f(inp["x"])
    c = f(inp["c"])
    fm = lambda v: np.ascontiguousarray(v.reshape(-1, 128).T)
    pcols = np.zeros((128, 2 * NPL), np.float32)
    b_ada, ng, b_in = f(inp["b_ada"]), f(inp["norm_gain"]), f(inp["b_in"])
    conv_b, lcg, lcb, gg = f(inp["conv_b"]), f(inp["ln_conv_gain"]), f(inp["ln_conv_bias"]), f(inp["group_gain"])
    conv_w, b_sp = f(inp["conv_w"]), f(inp["b_spatial"])
    for l in range(2):
        o = l * NPL
        pcols[:, o:o + 48] = fm(b_ada[l])
        for i in range(4):
            pcols[:, o + 48 + 8 * i:o + 56 + 8 * i] = fm(ng[l, i])
        pcols[:, o + 80:o + 88] = fm(b_in[l, 1024:])
        pcols[:, o + 88:o + 92] = fm(conv_b[l])
        pcols[:, o + 92:o + 96] = fm(lcg[l])
        pcols[:, o + 96:o + 100] = fm(lcb[l])
        pcols[:, o + 100:o + 104] = fm(gg[l, 512:])
        cw = conv_w[l].reshape(31, 4, 128)
        pcols[:, o + 104:o + 228] = cw.transpose(2, 0, 1).reshape(128, 124)
        pcols[:, o + 228:o + 236] = b_sp[l].T
    rowvecs = np.stack([np.stack([f(inp["ln_v_gain"])[l], f(inp["ln_v_bias"])[l], gg[l, :512]]) for l in range(2)])
    wsT = np.ascontiguousarray(f(inp["w_spatial"]).transpose(0, 3, 1, 2))
    shared = {
        "pcols": pcols,
        "ident": np.eye(128, dtype=np.float32),
        "w_ada": f(inp["w_ada"]),
        "w_in": f(inp["w_in"]),
        "w_out": f(inp["w_out"]),
        "b_in_row": np.ascontiguousarray(b_in[:, :1024]),
        "rowvecs": np.ascontiguousarray(rowvecs),
        "wsT": wsT,
        "ffn_wg": f(inp["ffn_w_gate"]),
        "ffn_wu": f(inp["ffn_w_up"]),
        "ffn_wd": f(inp["ffn_w_down"]),
        "router_w": f(inp["router_w"])[0],
        "router_b": f(inp["router_b"])[0],
        "moe_wg": f(inp["moe_w_gate"])[0],
        "moe_wu": f(inp["moe_w_up"])[0],
        "moe_wd": f(inp["moe_w_down"])[0],
    }
    maps = []
    for b in range(8):
        m = dict(shared)
        m["xT"] = np.ascontiguousarray(x[b].T)
        m["cT"] = fm(c[b])
        maps.append(m)
    return maps


_NC_CACHE = {}


def kernel(**inputs):
    maps = prep_inputs(inputs)
    if "nc" not in _NC_CACHE:
        _NC_CACHE["nc"] = build_program()
    res = run_bass_kernel_spmd(_NC_CACHE["nc"], maps, core_ids=list(range(8)))
    out = np.stack([np.ascontiguousarray(r["outT"].T) for r in res.results]).astype(np.float32)
    return out
```

```python
from contextlib import ExitStack

import numpy as np
import concourse.bass as bass
import concourse.mybir as mybir
from concourse.bass_utils import run_bass_kernel_spmd

F32 = mybir.dt.float32
BF16 = mybir.dt.bfloat16
AF = mybir.ActivationFunctionType
ALU = mybir.AluOpType
AX = mybir.AxisListType

D = 1024
T = 2048
KC = 8
DFF = 3584
NFC = 28
NE = 8
EPS = 1e-6
MT = 256
NMT = T // MT
FT = 512
NFT = T // FT
NPL = 236
COMPUTE = ("pe", "act", "dve", "pool")


class Op:
    __slots__ = ("eng", "fn", "deps", "sig", "val", "semkey", "nosync", "gi")


class Sched:
    def __init__(self):
        self.ops = {e: [] for e in ("pe", "act", "dve", "pool", "sp")}
        self.lastw = {}
        self.readers = {}
        self.n = 0
        self.dmas = []

    def add(self, eng, fn, reads=(), writes=(), dma=None, nosync=False):
        op = Op()
        op.eng = eng
        op.fn = fn
        op.sig = False
        op.val = None
        op.semkey = dma
        op.nosync = nosync
        op.gi = self.n
        deps = {}
        for k in reads:
            w = self.lastw.get(k)
            if w is not None:
                deps[id(w)] = w
        for k in writes:
            w = self.lastw.get(k)
            if w is not None:
                deps[id(w)] = w
            rd = self.readers.get(k)
            if rd:
                for r in rd.values():
                    deps[id(r)] = r
        op.deps = list(deps.values())
        for k in writes:
            self.lastw[k] = op
            self.readers[k] = {}
        for k in reads:
            rk = eng if dma is None else ("dma", self.n)
            self.readers.setdefault(k, {})[rk] = op
        self.ops[eng].append(op)
        if dma is not None:
            self.dmas.append(op)
        self.n += 1
        return op

    def barrier(self):
        lasts = []
        for lst in self.ops.values():
            for op in reversed(lst):
                if op.semkey is None and op.fn is not None:
                    lasts.append(op)
                    break
        deps = lasts + self.dmas
        for e in self.ops:
            op = Op()
            op.eng = e
            op.fn = None
            op.sig = False
            op.val = None
            op.semkey = None
            op.nosync = False
            op.gi = self.n
            op.deps = [d for d in deps]
            self.ops[e].append(op)
        self.dmas = []
        self.lastw = {}
        self.readers = {}

    def emit(self, nc, final_wait_ops=()):
        for e, lst in self.ops.items():
            for op in lst:
                for d in op.deps:
                    if d.semkey is None:
                        if d.eng == op.eng and op.semkey is None and op.nosync:
                            continue
                        d.sig = True
        cnt = {e: 0 for e in COMPUTE}
        dcnt = {}
        for e, lst in self.ops.items():
            for op in lst:
                if op.semkey is not None:
                    dcnt[op.semkey] = dcnt.get(op.semkey, 0) + 16
                    op.val = dcnt[op.semkey]
                elif op.sig:
                    cnt[op.eng] += 1
                    op.val = cnt[op.eng]
        with ExitStack() as st:
            sems = {e: st.enter_context(nc.semaphore("s_" + e)) for e in COMPUTE}
            dsems = {k: st.enter_context(nc.semaphore("d_" + str(i))) for i, k in enumerate(dcnt)}
            block = st.enter_context(nc.Block())
            sched = self

            def needs_of(op):
                need = {}
                for d in op.deps:
                    if d.semkey is not None:
                        s = dsems[d.semkey]
                    else:
                        if not d.sig:
                            continue
                        if d.eng == op.eng and op.semkey is None and op.nosync:
                            continue
                        s = sems[d.eng]
                    cur = need.get(s.num)
                    if cur is None or cur[1] < d.val:
                        need[s.num] = (s, d.val, d.gi)
                return need

            def run(eng_name, eng):
                known = {}
                lst = sched.ops[eng_name]
                needs = [needs_of(op) for op in lst]
                look = 6 if eng_name == "pe" else 0
                for j, op in enumerate(lst):
                    for num, (s, v, _) in needs[j].items():
                        if known.get(num, 0) < v:
                            vv = v
                            for jj in range(j + 1, min(len(lst), j + 1 + look)):
                                nx = needs[jj].get(num)
                                if nx is not None and nx[2] < op.gi and nx[1] > vv:
                                    vv = nx[1]
                            eng.wait_ge(s, vv)
                            known[num] = vv
                    if op.fn is None:
                        continue
                    ins = op.fn(eng)
                    if op.semkey is not None:
                        ins.then_inc(dsems[op.semkey], 16)
                    elif op.sig:
                        ins.then_inc(sems[op.eng], 1)
                if eng_name == "sp":
                    for op in final_wait_ops:
                        eng.wait_ge(dsems[op.semkey], op.val)

            block.sync(lambda e: run("sp", e))
            block.scalar(lambda e: run("act", e))
            block.vector(lambda e: run("dve", e))
            block.gpsimd(lambda e: run("pool", e))
            block.tensor(lambda e: run("pe", e))


class Arena:
    def __init__(self, ap, nbytes):
        self.ap = ap
        self.nbytes = nbytes
        self.off = 0

    def reset(self):
        self.off = 0

    def alloc(self, free_shape, dtype):
        esz = 4 if dtype == F32 else 2
        n = int(np.prod(free_shape)) * esz
        n_al = (n + 31) // 32 * 32
        assert self.off + n_al <= self.nbytes, ("arena overflow", self.off, n_al, self.nbytes)
        v = self.ap[:, self.off // 2:(self.off + n) // 2]
        self.off += n_al
        if dtype == F32:
            v = v.bitcast(F32)
        if len(free_shape) == 2:
            v = v.rearrange("p (a b) -> p a b", b=free_shape[1])
        elif len(free_shape) == 3:
            v = v.rearrange("p (a b c) -> p a b c", b=free_shape[1], c=free_shape[2])
        return v


def build_program(n_layers=2, stop_after=None):
    nc = bass.Bass("TRN2", target_bir_lowering=False)

    def din(name, shape):
        return nc.dram_tensor(name, list(shape), F32, kind="ExternalInput").ap()

    xT_d = din("xT", [D, T])
    cT_d = din("cT", [128, KC])
    pcols_d = din("pcols", [128, 2 * NPL])
    ident_d = din("ident", [128, 128])
    w_ada_d = din("w_ada", [2, D, 6 * D])
    w_in_d = din("w_in", [2, D, 2 * D])
    w_out_d = din("w_out", [2, D, D])
    b_in_row_d = din("b_in_row", [2, D])
    rowvecs_d = din("rowvecs", [2, 3, 512])
    wsT_d = din("wsT", [2, 128, 8, 128])
    ffn_wg_d = din("ffn_wg", [1, D, DFF])
    ffn_wu_d = din("ffn_wu", [1, D, DFF])
    ffn_wd_d = din("ffn_wd", [1, DFF, D])
    rw_d = din("router_w", [D, NE])
    rb_d = din("router_b", [NE])
    moe_wg_d = din("moe_wg", [NE, D, DFF])
    moe_wu_d = din("moe_wu", [NE, D, DFF])
    moe_wd_d = din("moe_wd", [NE, DFF, D])
    out_d = nc.dram_tensor("outT", [D, T], F32, kind="ExternalOutput").ap()

    ARENA_BYTES = 132 * 1024
    with ExitStack() as st:
        def sb(name, shape, dt):
            return st.enter_context(nc.sbuf_tensor(name, list(shape), dt))

        xT = sb("xT_sb", [128, KC, T], F32)
        pcols = sb("pcols_sb", [128, 2 * NPL], F32)
        modt = sb("modt", [128, 2, 48], F32)
        dcols = sb("dcols", [128, 2, 32], F32)
        ident32 = sb("ident32", [128, 128], F32)
        identbf = sb("identbf", [128, 128], BF16)
        onesD = sb("onesD", [128, 128], BF16)
        ones512 = sb("ones512", [128, 128], BF16)
        ones32_512 = sb("ones32_512", [128, 128], F32)
        ones32 = sb("ones32", [128, 128], F32)
        onesrow = sb("onesrow", [1, 128], BF16)
        epsc = sb("epsc", [128, 1], F32)
        cwh = sb("cwh", [128, 124], F32)
        eps4 = sb("eps4", [128, 1], F32)
        cst = sb("cst", [128, KC], F32)
        cact = sb("cact", [128, KC], BF16)
        small = sb("small", [128, 64], F32)
        lg = sb("lg", [128, 16, NE], F32)
        lg2 = sb("lg2", [128, 16, NE], F32)
        eq1 = sb("eq1", [128, 16, NE], F32)
        eq2 = sb("eq2", [128, 16, NE], F32)
        gates = sb("gates", [128, 16, NE], F32)
        rt = sb("rt", [128, 6, 16], F32)
        rw32 = sb("rw32", [128, KC, NE], F32)
        rb_bc = sb("rb_bc", [128, NE], F32)
        arena_t = sb("arena", [128, ARENA_BYTES // 2], BF16)
        ps = st.enter_context(nc.psum_tensor("ps", [128, 8, 512], F32))
        A = Arena(arena_t[:], ARENA_BYTES)
        S = Sched()

        def ACT(out, in_, func, r, w, bias=None, scale=None, accum=None, nosync=False):
            kw = {}
            if bias is not None:
                kw["bias"] = bias
            if scale is not None:
                kw["scale"] = scale
            if accum is not None:
                kw["accum_out"] = accum
            return S.add("act", lambda e: e.activation(out=out, in_=in_, func=func, **kw), r, w, nosync=nosync)

        def TT(eng, out, in0, in1, op, r, w, nosync=False):
            return S.add(eng, lambda e: e.tensor_tensor(out=out, in0=in0, in1=in1, op=op), r, w, nosync=nosync)

        def STT(eng, out, in0, scalar, in1, op0, op1, r, w, nosync=False):
            return S.add(eng, lambda e: e.scalar_tensor_tensor(out=out, in0=in0, scalar=scalar, in1=in1, op0=op0, op1=op1), r, w, nosync=nosync)

        def TS(eng, out, in0, s1, s2, op0, op1, r, w, nosync=False):
            if s2 is None:
                return S.add(eng, lambda e: e.tensor_scalar(out=out, in0=in0, scalar1=s1, scalar2=None, op0=op0), r, w, nosync=nosync)
            return S.add(eng, lambda e: e.tensor_scalar(out=out, in0=in0, scalar1=s1, scalar2=s2, op0=op0, op1=op1), r, w, nosync=nosync)

        def MM(out, lhsT, rhs, start, stop, r, w):
            return S.add("pe", lambda e: e.matmul(out, lhsT, rhs, start=start, stop=stop), r, w, nosync=True)

        def DMA(eng, out, in_, r, w, key):
            return S.add(eng, lambda e: e.dma_start(out=out, in_=in_), r, w, dma=key)

        def MEMSET(eng, ap, val, w):
            return S.add(eng, lambda e: e.memset(ap, val), (), w)

        def rsqrt_tile(out_sb, in_ps, r, w, wkey_tmp):
            ACT(out_sb, in_ps, AF.Sqrt, r + ["epsc"], w, bias=epsc[:, 0:1])
            S.add("dve", lambda e: e.reciprocal(out=out_sb, in_=out_sb), w, w)

        DMA("sp", pcols[:], pcols_d, [], ["pcols"], "pcols")
        DMA("sp", cst[:], cT_d, [], ["cst"], "cst")
        DMA("sp", ident32[:], ident_d, [], ["ident32"], "ident")
        for k in range(KC):
            DMA("sp", xT[:, k, :], xT_d[k * 128:(k + 1) * 128, :], [], [("x", k, mt) for mt in range(NMT)], ("xin", k))
        MEMSET("dve", onesD[:], 1.0 / D, ["onesD"])
        MEMSET("dve", ones512[:], 1.0 / 512, ["ones512"])
        MEMSET("dve", ones32_512[:], 1.0 / 512, ["ones32_512"])
        MEMSET("dve", ones32[:], 1.0, ["ones32"])
        MEMSET("dve", onesrow[:], 1.0, ["onesrow"])
        MEMSET("dve", epsc[:], EPS, ["epsc"])
        MEMSET("dve", eps4[:], 4.0 * EPS, ["eps4"])
        ACT(identbf[:], ident32[:], AF.Copy, ["ident32"], ["identbf"])
        ACT(cact[:], cst[:], AF.Silu, ["cst"], ["cact"])

        def pc(l, c0, n=1):
            return pcols[:, l * NPL + c0:l * NPL + c0 + n]

        def mod_dma(l, pi, stage):
            ns = len(stage)
            sl = stage[pi % ns]
            DMA("pool", sl, w_ada_d[l, :, pi * 256:(pi + 1) * 256].rearrange("(k p) n -> p k n", p=128), [], [("wada", pi % ns)], ("wada", pi % ns))

        def mod_mm(l, pi, stage):
            ns = len(stage)
            sl = stage[pi % ns]
            for o in range(2):
                oc = pi * 2 + o
                for k in range(KC):
                    MM(ps[:, 5, 256 + oc:256 + oc + 1], sl[:, k, o * 128:(o + 1) * 128], cact[:, k:k + 1], k == 0, k == KC - 1,
                       [("wada", pi % ns), "cact"], ["psmod"])

        def mod_fin(l):
            TT("dve", modt[:, l, :], ps[:, 5, 256:304], pc(l, 0, 48), ALU.add, ["psmod", "pcols"], [("mod", l)])
            STT("dve", dcols[:, l, 0:8], modt[:, l, 8:16], 1.0, pc(l, 48, 8), ALU.add, ALU.mult, [("mod", l), "pcols"], [("dc", l, 0)])
            TT("dve", dcols[:, l, 8:16], modt[:, l, 16:24], pc(l, 56, 8), ALU.mult, [("mod", l), "pcols"], [("dc", l, 1)])
            STT("dve", dcols[:, l, 16:24], modt[:, l, 32:40], 1.0, pc(l, 64, 8), ALU.add, ALU.mult, [("mod", l), "pcols"], [("dc", l, 2)])
            TT("dve", dcols[:, l, 24:32], modt[:, l, 40:48], pc(l, 72, 8), ALU.mult, [("mod", l), "pcols"], [("dc", l, 3)])

        def norm_mod_tile(l, which, c0, ncol, sq_bufs, rstd_buf, tmp_bufs, out_fn, xkeys_fn, stat_bank, h32_bufs=None, post=None):
            gsc = 0 if which == 1 else 16
            shc = 0 if which == 1 else 24
            for k in range(KC):
                sq = sq_bufs[k % 2]
                ACT(sq, xT[:, k, c0:c0 + ncol], AF.Square, [xkeys_fn(k)], [("sqA", k % 2)])
                MM(ps[:, stat_bank, 0:ncol], onesD[:], sq, k == 0, k == KC - 1, [("sqA", k % 2), "onesD"], [("ps", stat_bank)])
            rsqrt_tile(rstd_buf, ps[:, stat_bank, 0:ncol], [("ps", stat_bank)], ["rstdA"], "rstdA_t")
            for k in range(KC):
                tmp = tmp_bufs[k % 2]
                TT("dve", tmp, xT[:, k, c0:c0 + ncol], rstd_buf, ALU.mult, [xkeys_fn(k), "rstdA"], [("tmpA", k % 2)])
                dst, dkeys = out_fn(k)
                if h32_bufs is None:
                    ACT(dst, tmp, AF.Identity, [("tmpA", k % 2), ("dc", l, 0 if which == 1 else 2), ("mod", l)], dkeys,
                        scale=dcols[:, l, gsc + k:gsc + k + 1], bias=modt[:, l, shc + k:shc + k + 1])
                else:
                    h32 = h32_bufs[k % 2]
                    ACT(h32, tmp, AF.Identity, [("tmpA", k % 2), ("dc", l, 2), ("mod", l)], [("h32", k % 2)],
                        scale=dcols[:, l, gsc + k:gsc + k + 1], bias=modt[:, l, shc + k:shc + k + 1])
                    S.add("pool", lambda e, dst=dst, h32=h32: e.tensor_copy(out=dst, in_=h32), [("h32", k % 2)], dkeys)
                    post(k, h32, ("h32", k % 2))

        def mixer(l, do_mod_next):
            A.reset()
            w_in = A.alloc([KC, 2 * D], BF16)
            w_out = A.alloc([KC, D], BF16)
            hb = A.alloc([KC, MT], BF16)
            ycat = [A.alloc([KC, MT], BF16) for _ in range(3)]
            wsT = A.alloc([8, 128], BF16)
            bct = A.alloc([3, 512], F32)
            binrow = A.alloc([D], BF16)
            ub = [A.alloc([512], F32) for _ in range(2)]
            gv = A.alloc([512], F32)
            vb = [A.alloc([512], BF16) for _ in range(2)]
            yanb = [A.alloc([512], BF16) for _ in range(2)]
            junk = A.alloc([512], BF16)
            sg = A.alloc([MT], F32)
            abb = junk.bitcast(F32)
            xg = [A.alloc([4, 30 + MT], F32) for _ in range(2)]
            ycb = [A.alloc([4, MT], F32) for _ in range(2)]
            sqc = [A.alloc([MT], F32) for _ in range(2)]
            mean_sb = A.alloc([MT], F32)
            m2 = A.alloc([MT], F32)
            sqA = [A.alloc([MT], BF16) for _ in range(2)]
            sqB = [A.alloc([MT], BF16) for _ in range(2)]
            rstdA = A.alloc([MT], F32)
            tmpA = [A.alloc([MT], F32) for _ in range(2)]
            yo = A.alloc([KC, MT], F32)
            sqF = [A.alloc([MT], BF16) for _ in range(2)]
            rsF = A.alloc([MT], F32)
            tmpF = [A.alloc([MT], F32) for _ in range(2)]
            stage = [A.alloc([KC, 256], BF16) for _ in range(2)]

            DMA("pool", w_in, w_in_d[l].rearrange("(k p) n -> p k n", p=128), [], ["w_in"], "w_in")
            DMA("pool", wsT, wsT_d[l], [], ["wsT_raw"], "wsT")
            DMA("pool", binrow[0:1, :], b_in_row_d[l:l + 1, :], [], ["binrow"], "binrow")
            DMA("sp", bct, rowvecs_d[l].partition_broadcast(128), [], ["bct"], "bct")
            DMA("pool", w_out, w_out_d[l].rearrange("(k p) n -> p k n", p=128), [], ["w_out"], "w_out")
            MEMSET("dve", wsT[64:128, :, 0:64], 0.0, ["wsT_raw"])
            MEMSET("dve", xg[0][:, :, 0:30], 0.0, [("xg_halo", 0)])
            TS("dve", small[:, 32:36], pc(l, 84, 4), 0.5, None, ALU.mult, None, ["pcols"], ["hbcol"])
            TS("dve", cwh[:], pc(l, 104, 124), 0.5, None, ALU.mult, None, ["pcols"], ["cwh"])
            zi = [0]
            fi = [0]

            def zbank():
                zi[0] += 1
                return zi[0] % 2

            def fbank():
                fi[0] += 1
                return 2 + fi[0] % 2

            NB = MT // 128

            def rsq(out_sb, in_ap, r, w, e4=False):
                ACT(out_sb, in_ap, AF.Sqrt, r + ["epsc", "eps4"], w, bias=(eps4 if e4 else epsc)[:, 0:1])
                S.add("dve", lambda e: e.reciprocal(out=out_sb, in_=out_sb), w, w)

            def front(mt):
                c0 = mt * MT
                yb = ycat[mt % 3]
                xgb = xg[mt % 2]
                hk = lambda k: ("hT", k)
                if do_mod_next:
                    mod_dma(l + 1, 3 * mt, stage)
                    mod_dma(l + 1, 3 * mt + 1, stage)
                for k in range(KC):
                    sq = sqA[k % 2]
                    ACT(sq, xT[:, k, c0:c0 + MT], AF.Square, [("x", k, mt)], [("sqA", k % 2)])
                    MM(ps[:, 4, 0:MT], onesD[:], sq, k == 0, k == KC - 1, [("sqA", k % 2), "onesD"], [("ps", 4)])
                    yield
                rsq(rstdA, ps[:, 4, 0:MT], [("ps", 4)], ["rstdA"])
                yield
                for k in range(KC):
                    tmp = tmpA[k % 2]
                    TT("dve", tmp, xT[:, k, c0:c0 + MT], rstdA, ALU.mult, [("x", k, mt), "rstdA"], [("tmpA", k % 2)])
                    ACT(hb[:, k, :], tmp, AF.Identity, [("tmpA", k % 2), ("dc", l, 0), ("mod", l)], [hk(k)],
                        scale=dcols[:, l, k:k + 1], bias=modt[:, l, k:k + 1])
                    yield
                for bi in range(NB):
                    tb = mt * NB + bi
                    b0 = bi * 128
                    pb = tb % 2
                    u = ub[pb]
                    v = vb[pb]
                    yan = yanb[pb]
                    bu = zbank()
                    for k in range(KC):
                        MM(ps[:, bu, :], hb[:, k, b0:b0 + 128], w_in[:, k, 0:512], k == 0, False, [hk(k), "w_in"], [("ps", bu)])
                    MM(ps[:, bu, :], onesrow[0:1, :], binrow[0:1, 0:512], False, True, ["onesrow", "binrow"], [("ps", bu)])
                    yield
                    bv = zbank()
                    for k in range(KC):
                        MM(ps[:, bv, :], hb[:, k, b0:b0 + 128], w_in[:, k, 512:1024], k == 0, False, [hk(k), "w_in"], [("ps", bv)])
                    MM(ps[:, bv, :], onesrow[0:1, :], binrow[0:1, 512:1024], False, True, ["onesrow", "binrow"], [("ps", bv)])
                    yield
                    ACT(u, ps[:, bu, :], AF.Gelu_apprx_tanh, [("ps", bu)], [("u", pb)])
                    yield
                    ACT(gv, ps[:, bv, :], AF.Gelu_apprx_tanh, [("ps", bv)], ["gv"])
                    yield
                    s0 = pb * 16
                    S.add("dve", lambda e, s0=s0: e.bn_stats(out=small[:, s0:s0 + 6], in_=gv), ["gv"], [("st6", pb)])
                    S.add("dve", lambda e, s0=s0: e.bn_aggr(out=small[:, s0 + 6:s0 + 8], in_=small[:, s0:s0 + 6]), [("st6", pb)], [("mv", pb)])
                    yield
                    ACT(small[:, s0 + 8:s0 + 9], small[:, s0 + 7:s0 + 8], AF.Sqrt, [("mv", pb), "epsc"], [("sd", pb)], bias=epsc[:, 0:1])
                    S.add("dve", lambda e, s0=s0: e.reciprocal(out=small[:, s0 + 9:s0 + 10], in_=small[:, s0 + 8:s0 + 9]), [("sd", pb)], [("rv", pb)])
                    yield
                    STT("dve", small[:, s0 + 10:s0 + 11], small[:, s0 + 6:s0 + 7], -1.0, small[:, s0 + 9:s0 + 10], ALU.mult, ALU.mult,
                        [("mv", pb), ("rv", pb)], [("nb", pb)])
                    yield
                    ACT(gv, gv, AF.Identity, ["gv", ("rv", pb), ("nb", pb)], ["gv"],
                        scale=small[:, s0 + 9:s0 + 10], bias=small[:, s0 + 10:s0 + 11])
                    yield
                    TT("dve", gv, gv, bct[:, 0, :], ALU.mult, ["gv", "bct"], ["gv"])
                    yield
                    TT("dve", v, gv, bct[:, 1, :], ALU.add, ["gv", "bct"], [("v", pb)])
                    yield
                    bm = 4
                    for h in range(8):
                        MM(ps[:, bm, h * 64:(h + 1) * 64], wsT[:, h, :], v[:, h * 64:(h + 1) * 64], True, True,
                           ["wsT_raw", ("v", pb)], [("ps", bm)])
                    yield
                    for h in range(8):
                        STT("dve", u[:, h * 64:(h + 1) * 64], ps[:, bm, h * 64:(h + 1) * 64], pc(l, 228 + h), u[:, h * 64:(h + 1) * 64],
                            ALU.add, ALU.mult, [("ps", bm), ("u", pb), "pcols"], [("u", pb)], nosync=(h > 0))
                        if h % 2 == 1:
                            yield
                    MEMSET("dve", small[:, s0 + 11:s0 + 12], 0.0, [("ssA", pb)])
                    ACT(junk, u, AF.Square, [("u", pb), ("ssA", pb)], ["junk", ("ssA", pb)], accum=small[:, s0 + 11:s0 + 12])
                    yield
                    TS("dve", small[:, s0 + 12:s0 + 13], small[:, s0 + 11:s0 + 12], 1.0 / 512, EPS, ALU.mult, ALU.add, [("ssA", pb)], [("ssA2", pb)])
                    ACT(small[:, s0 + 13:s0 + 14], small[:, s0 + 12:s0 + 13], AF.Sqrt, [("ssA2", pb)], [("sdA", pb)])
                    S.add("dve", lambda e, s0=s0: e.reciprocal(out=small[:, s0 + 14:s0 + 15], in_=small[:, s0 + 13:s0 + 14]), [("sdA", pb)], [("rsA", pb)])
                    yield
                    STT("dve", yan, u, small[:, s0 + 14:s0 + 15], bct[:, 2, :], ALU.mult, ALU.mult, [("u", pb), ("rsA", pb), "bct"], [("yan", pb)])
                    yield
                    psT = ps[:, 5, 0:256].bitcast(BF16).rearrange("p (j t) -> p j t", t=128)
                    for j in range(4):
                        S.add("pe", lambda e, j=j, yan=yan, psT=psT: e.transpose(out=psT[:, j, :], in_=yan[:, j * 128:(j + 1) * 128], identity=identbf[:]),
                              [("yan", pb), "identbf"], [("ps", 5)], nosync=True)
                    ACT(yb[:, 0:4, b0:b0 + 128], psT, AF.Copy, [("ps", 5)], [("yc", mt % 3, j, bi) for j in range(4)])
                    yield
                if mt > 0:
                    S.add("pool", lambda e, xgb=xgb, prev=xg[(mt - 1) % 2]: e.tensor_copy(out=xgb[:, :, 0:30], in_=prev[:, :, MT:MT + 30]),
                          [("xg", (mt - 1) % 2, j) for j in range(4)], [("xg_halo", mt % 2)])
                for j in range(4):
                    ba = zbank()
                    for k in range(KC):
                        MM(ps[:, ba, 0:MT], w_in[:, k, (8 + j) * 128:(9 + j) * 128], hb[:, k, :], k == 0, k == KC - 1, [hk(k), "w_in"], [("ps", ba)])
                    yield
                    bg = zbank()
                    for k in range(KC):
                        MM(ps[:, bg, 0:MT], w_in[:, k, (12 + j) * 128:(13 + j) * 128], hb[:, k, :], k == 0, k == KC - 1, [hk(k), "w_in"], [("ps", bg)])
                    yield
                    ACT(sg, ps[:, bg, 0:MT], AF.Tanh, [("ps", bg), "hbcol"], ["sg"], scale=0.5, bias=small[:, 32 + j:33 + j])
                    ACT(abb, ps[:, ba, 0:MT], AF.Identity, [("ps", ba), "pcols"], ["junk"], bias=pc(l, 80 + j))
                    yield
                    STT("dve", xgb[:, j, 30:30 + MT], sg, 1.0, abb, ALU.add, ALU.mult,
                        ["junk", "sg"], [("xg", mt % 2, j)])
                    yield
                if do_mod_next:
                    mod_mm(l + 1, 3 * mt, stage)
                    yield
                    mod_dma(l + 1, 3 * mt + 2, stage)
                    mod_mm(l + 1, 3 * mt + 1, stage)
                    yield
                    mod_mm(l + 1, 3 * mt + 2, stage)
                    yield

            def conv(mt):
                xgb = xg[mt % 2]
                yc = ycb[mt % 2]
                xr = lambda j: [("xg", mt % 2, j), ("xg_halo", mt % 2)]
                for j in range(4):
                    TS("dve", yc[:, j, :], xgb[:, j, 0:MT], cwh[:, j:j + 1], pc(l, 88 + j), ALU.mult, ALU.add,
                       xr(j) + ["pcols", "cwh"], [("ycv", mt % 2, j)])
                    yield
                    for tap in range(1, 31):
                        STT("dve", yc[:, j, :], xgb[:, j, tap:tap + MT], cwh[:, tap * 4 + j:tap * 4 + j + 1], yc[:, j, :], ALU.mult, ALU.add,
                            xr(j) + [("ycv", mt % 2, j)], [("ycv", mt % 2, j)], nosync=True)
                        if tap % 2 == 0:
                            yield

            def post(mt):
                c0 = mt * MT
                yb = ycat[mt % 3]
                yc = ycb[mt % 2]
                yk = lambda j: ("ycv", mt % 2, j)
                for j in range(4):
                    MM(ps[:, 6, 0:MT], ones32_512[:], yc[:, j, :], j == 0, j == 3, [yk(j), "ones32_512"], [("ps", 6)])
                yield
                for j in range(4):
                    ACT(sqc[j % 2], yc[:, j, :], AF.Square, [yk(j)], [("sqc", j % 2)])
                    MM(ps[:, 7, 0:MT], ones32_512[:], sqc[j % 2], j == 0, j == 3, [("sqc", j % 2), "ones32_512"], [("ps", 7)])
                    yield
                ACT(mean_sb, ps[:, 6, 0:MT], AF.Copy, [("ps", 6)], ["mean_sb"])
                yield
                TT("dve", m2, mean_sb, mean_sb, ALU.mult, ["mean_sb"], ["m2"])
                yield
                TT("dve", m2, ps[:, 7, 0:MT], m2, ALU.subtract, [("ps", 7), "m2"], ["m2"])
                yield
                rsq(m2, m2, ["m2"], ["m2"])
                yield
                for j in range(4):
                    TT("pool", yc[:, j, :], yc[:, j, :], mean_sb, ALU.subtract, [yk(j), "mean_sb"], [yk(j)])
                    yield
                    TT("pool", yc[:, j, :], yc[:, j, :], m2, ALU.mult, [yk(j), "m2"], [yk(j)])
                    yield
                    ACT(yc[:, j, :], yc[:, j, :], AF.Identity, [yk(j), "pcols"], [yk(j)], scale=pc(l, 92 + j), bias=pc(l, 96 + j))
                    yield
                    ACT(sqc[j % 2], yc[:, j, :], AF.Tanh, [yk(j)], [("sqc", j % 2)], scale=0.5)
                    yield
                    STT("dve", yc[:, j, :], sqc[j % 2], 1.0, yc[:, j, :], ALU.add, ALU.mult, [yk(j), ("sqc", j % 2)], [yk(j)])
                    yield
                    ACT(sqB[j % 2], yc[:, j, :], AF.Square, [yk(j)], [("sqB", j % 2)])
                    MM(ps[:, 6, 0:MT], ones512[:], sqB[j % 2], j == 0, j == 3, [("sqB", j % 2), "ones512"], [("ps", 6)])
                    yield
                rsq(m2, ps[:, 6, 0:MT], [("ps", 6)], ["m2"], e4=True)
                yield
                for j in range(4):
                    TT("dve", yc[:, j, :], yc[:, j, :], m2, ALU.mult, [yk(j), "m2"], [yk(j)])
                    ACT(yb[:, 4 + j, :], yc[:, j, :], AF.Identity, [yk(j), "pcols"], [("yc", mt % 3, 4 + j, bi) for bi in range(NB)],
                        scale=pc(l, 100 + j))
                    yield
                ykeys = [[("yc", mt % 3, k, bi) for bi in range(NB)] for k in range(KC)]
                for dc in range(KC):
                    bo = fbank()
                    for k in range(KC):
                        MM(ps[:, bo, 0:MT], w_out[:, k, dc * 128:(dc + 1) * 128], yb[:, k, :], k == 0, k == KC - 1, ykeys[k] + ["w_out"], [("ps", bo)])
                    yield
                    ACT(yo[:, dc, :], ps[:, bo, 0:MT], AF.Copy, [("ps", bo)], [("yo", dc)])
                    ACT(sqF[dc % 2], ps[:, bo, 0:MT], AF.Square, [("ps", bo)], [("sqF", dc % 2)])
                    MM(ps[:, 7, 0:MT], onesD[:], sqF[dc % 2], dc == 0, dc == KC - 1, [("sqF", dc % 2), "onesD"], [("ps", 7)])
                    yield
                rsq(rsF, ps[:, 7, 0:MT], [("ps", 7)], ["rsF"])
                yield
                for dc in range(KC):
                    STT("dve", tmpF[dc % 2], yo[:, dc, :], dcols[:, l, 8 + dc:9 + dc], rsF, ALU.mult, ALU.mult,
                        [("yo", dc), "rsF", ("dc", l, 1)], [("tmpF", dc % 2)])
                    TT("pool", xT[:, dc, c0:c0 + MT], xT[:, dc, c0:c0 + MT], tmpF[dc % 2], ALU.add, [("x", dc, mt), ("tmpF", dc % 2)], [("x", dc, mt)])
                    yield

            def drive(gens):
                gens = [g for g in gens if g is not None]
                while gens:
                    for g in list(gens):
                        try:
                            next(g)
                        except StopIteration:
                            gens.remove(g)

            drive([front(0)])
            drive([conv(0), front(1)])
            for mt in range(1, NMT):
                drive([conv(mt), front(mt + 1) if mt + 1 < NMT else None, post(mt - 1)])
            drive([post(NMT - 1)])
            if do_mod_next:
                mod_fin(l + 1)
            S.barrier()

        def ffn(l, moe):
            A.reset()
            hT = A.alloc([KC, T], BF16)
            acc = A.alloc([KC, T], F32)
            acc_off = 32768
            wg = [A.alloc([KC, 256], BF16) for _ in range(2)]
            wu = [A.alloc([KC, 256], BF16) for _ in range(2)]
            wd = [A.alloc([2, D], BF16) for _ in range(2)]
            wslot_off = 32768 + 65536
            asl = [[A.alloc([FT], BF16) for _ in range(2)] for _ in range(2)]
            gate_bc = A.alloc([T], BF16)
            tsl = [A.alloc([FT], BF16) for _ in range(2)]
            dg = A.alloc([4, 128], F32)
            A2 = Arena(arena_t[:], acc_off + 65536)
            A2.off = acc_off
            sqA = [A2.alloc([FT], BF16) for _ in range(2)]
            rstdA = A2.alloc([FT], F32)
            tmpA = [A2.alloc([FT], F32) for _ in range(2)]
            h32b = [A2.alloc([FT], F32) for _ in range(2)]
            AG = Arena(arena_t[:], wslot_off + 24576)
            AG.off = wslot_off
            sqG = [AG.alloc([FT], BF16) for _ in range(2)]
            rsG = AG.alloc([FT], F32)
            tmpG = [AG.alloc([FT], F32) for _ in range(2)]

            if moe:
                DMA("sp", rw32[:], rw_d.rearrange("(k p) e -> p k e", p=128), [], ["rw32"], "rw32")
                DMA("sp", rb_bc[:], rb_d.partition_broadcast(128), [], ["rb_bc"], "rb_bc")
            for ft in range(NFT):
                c0 = ft * FT
                xk = lambda k, ft=ft: ("x", k, ft)

                def post(k, h32, hkey, ft=ft):
                    for b in range(4):
                        MM(ps[:, b, 0:8], h32[:, b * 128:(b + 1) * 128], rw32[:, k, :], k == 0, k == KC - 1, [hkey, "rw32"], [("ps", b)])

                norm_mod_tile(l, 2, c0, FT, sqA, rstdA, tmpA,
                              lambda k, ft=ft: (hT[:, k, ft * FT:(ft + 1) * FT], [("hT", k, ft)]),
                              xk, 6, h32_bufs=h32b if moe else None, post=post if moe else None)
                if moe:
                    for b in range(4):
                        TT("dve", lg[:, ft * 4 + b, :], ps[:, b, 0:8], rb_bc[:], ALU.add, [("ps", b), "rb_bc"], [("lg", ft * 4 + b)])
            if moe:
                lgk = [("lg", i) for i in range(16)]
                bc3 = lambda ap: ap.unsqueeze(2).to_broadcast([128, 16, NE])
                S.add("dve", lambda e: e.tensor_reduce(out=rt[:, 0, :], in_=lg[:], axis=AX.X, op=ALU.max), lgk, ["m1"])
                TT("dve", eq1[:], lg[:], bc3(rt[:, 0, :]), ALU.is_equal, lgk + ["m1"], ["eq1"])
                STT("dve", lg2[:], eq1[:], -1e30, lg[:], ALU.mult, ALU.add, ["eq1"] + lgk, ["lg2"])
                S.add("dve", lambda e: e.tensor_reduce(out=rt[:, 1, :], in_=lg2[:], axis=AX.X, op=ALU.max), ["lg2"], ["m2r"])
                TT("dve", eq2[:], lg2[:], bc3(rt[:, 1, :]), ALU.is_equal, ["lg2", "m2r"], ["eq2"])
                TT("dve", rt[:, 2, :], rt[:, 1, :], rt[:, 0, :], ALU.subtract, ["m1", "m2r"], ["dm"])
                ACT(rt[:, 3, :], rt[:, 2, :], AF.Sigmoid, ["dm"], ["w1"], scale=-1.0)
                ACT(rt[:, 4, :], rt[:, 2, :], AF.Sigmoid, ["dm"], ["w2"])
                TT("dve", eq1[:], eq1[:], bc3(rt[:, 3, :]), ALU.mult, ["eq1", "w1"], ["eq1"])
                TT("dve", eq2[:], eq2[:], bc3(rt[:, 4, :]), ALU.mult, ["eq2", "w2"], ["eq2"])
                TT("dve", gates[:], eq1[:], eq2[:], ALU.add, ["eq1", "eq2"], ["gates"])
            S.barrier()

            experts = list(range(NE)) if moe else [0]
            wgd = moe_wg_d if moe else ffn_wg_d
            wud = moe_wu_d if moe else ffn_wu_d
            wdd = moe_wd_d if moe else ffn_wd_d
            steps = []
            gi = 0
            for e_ in experts:
                for g in range(NFC // 2):
                    for ft in range(NFT):
                        steps.append((e_, g, ft, gi))
                    gi += 1
            dbank = [0]
            first_acc = {}

            def down(step):
                e_, g, ft, gidx = step
                s = gidx % 2
                aslot = asl[step_index[step] % 2]
                for dc in range(KC):
                    b = 4 + dbank[0] % 4
                    dbank[0] += 1
                    MM(ps[:, b, :], wd[s][:, 0, dc * 128:(dc + 1) * 128], aslot[0], True, False, [("wd", s), ("a", step_index[step] % 2, 0)], [("ps", b)])
                    MM(ps[:, b, :], wd[s][:, 1, dc * 128:(dc + 1) * 128], aslot[1], False, True, [("wd", s), ("a", step_index[step] % 2, 1)], [("ps", b)])
                    dst = acc[:, dc, ft * FT:(ft + 1) * FT]
                    if (dc, ft) not in first_acc:
                        first_acc[(dc, ft)] = True
                        ACT(dst, ps[:, b, :], AF.Copy, [("ps", b)], [("acc", dc, ft)])
                    else:
                        TT("dve", dst, dst, ps[:, b, :], ALU.add, [("ps", b), ("acc", dc, ft)], [("acc", dc, ft)])
                    yield

            def load_group(step):
                e2, g2, _, gidx2 = step
                s2 = gidx2 % 2
                f0 = 2 * g2 * 128
                DMA("pool", wg[s2], wgd[e2, :, f0:f0 + 256].rearrange("(k p) n -> p k n", p=128), [], [("wg", s2)], ("wg", s2))
                DMA("pool", wu[s2], wud[e2, :, f0:f0 + 256].rearrange("(k p) n -> p k n", p=128), [], [("wu", s2)], ("wu", s2))
                DMA("pool", wd[s2], wdd[e2, f0:f0 + 256, :].rearrange("(f p) n -> p f n", p=128), [], [("wd", s2)], ("wd", s2))

            step_index = {st_: i for i, st_ in enumerate(steps)}
            prev = None
            cur_e = None
            for si, step in enumerate(steps):
                e_, g, ft, gidx = step
                s = gidx % 2
                if si == 0:
                    load_group(step)
                if moe and e_ != cur_e:
                    cur_e = e_
                    for t4 in range(NFT):
                        for b in range(4):
                            TS("dve", dg[:, b, :], ident32[:], gates[:, t4 * 4 + b, e_:e_ + 1], None, ALU.mult, None, ["gates", "ident32"], [("dg", b)])
                        MM(ps[:, 7, :], ones32[:], dg.rearrange("p a b -> p (a b)"), True, True, [("dg", b) for b in range(4)] + ["ones32"], [("ps", 7)])
                        ACT(gate_bc[:, t4 * FT:(t4 + 1) * FT], ps[:, 7, :], AF.Copy, [("ps", 7)], [("gbc", t4)])
                ai = si % 2
                dgen = down(prev) if prev is not None else iter(())

                def dn(n):
                    for _ in range(n):
                        next(dgen, None)

                for f in range(2):
                    bg_, bu_ = 2 * f, 2 * f + 1
                    for k in range(KC):
                        MM(ps[:, bg_, :], wg[s][:, k, f * 128:(f + 1) * 128], hT[:, k, ft * FT:(ft + 1) * FT], k == 0, k == KC - 1,
                           [("wg", s), ("hT", k, ft)], [("ps", bg_)])
                    ACT(tsl[f], ps[:, bg_, :], AF.Silu, [("ps", bg_)], [("ts", f)])
                    if f == 1:
                        dn(2)
                    for k in range(KC):
                        MM(ps[:, bu_, :], wu[s][:, k, f * 128:(f + 1) * 128], hT[:, k, ft * FT:(ft + 1) * FT], k == 0, k == KC - 1,
                           [("wu", s), ("hT", k, ft)], [("ps", bu_)])
                    if moe:
                        TT("dve", tsl[f], tsl[f], ps[:, bu_, :], ALU.mult, [("ts", f), ("ps", bu_)], [("ts", f)])
                        TT("pool", asl[ai][f], tsl[f], gate_bc[:, ft * FT:(ft + 1) * FT], ALU.mult, [("ts", f), ("gbc", ft)], [("a", ai, f)])
                    else:
                        TT("dve", asl[ai][f], tsl[f], ps[:, bu_, :], ALU.mult, [("ts", f), ("ps", bu_)], [("a", ai, f)])
                    dn(2)
                dn(8)
                prev = step
                if ft == 0 and si + NFT < len(steps):
                    load_group(steps[si + NFT])
            for _ in down(prev):
                pass
            S.barrier()
            for ft in range(NFT):
                c0 = ft * FT
                for dc in range(KC):
                    ACT(sqG[dc % 2], acc[:, dc, c0:c0 + FT], AF.Square, [("acc", dc, ft)], [("sqG", dc % 2)])
                    MM(ps[:, 6, :], onesD[:], sqG[dc % 2], dc == 0, dc == KC - 1, [("sqG", dc % 2), "onesD"], [("ps", 6)])
                rsqrt_tile(rsG, ps[:, 6, :], [("ps", 6)], ["rsG"], "rsG_t")
                for dc in range(KC):
                    STT("dve", tmpG[dc % 2], acc[:, dc, c0:c0 + FT], dcols[:, l, 24 + dc:25 + dc], rsG, ALU.mult, ALU.mult,
                        [("acc", dc, ft), "rsG", ("dc", l, 3)], [("tmpG", dc % 2)])
                    TT("pool", xT[:, dc, c0:c0 + FT], xT[:, dc, c0:c0 + FT], tmpG[dc % 2], ALU.add, [("x", dc, ft), ("tmpG", dc % 2)], [("x", dc, ft)])
            S.barrier()

        A.reset()
        stage0 = [A.alloc([KC, 256], BF16) for _ in range(3)]
        for pi in range(24):
            mod_dma(0, pi, stage0)
            mod_mm(0, pi, stage0)
        mod_fin(0)
        S.barrier()
        done = False
        for l in range(n_layers):
            mixer(l, do_mod_next=(l + 1 < n_layers))
            if stop_after == ("mixer", l):
                break
            ffn(l, moe=(l % 2 == 1))
            if stop_after == ("ffn", l):
                break
        outs = []
        for k in range(KC):
            outs.append(DMA("sp", out_d[k * 128:(k + 1) * 128, :], xT[:, k, :], [("x", k, i) for i in range(NMT)] + [("x", k, i) for i in range(NFT)], [], ("out", k)))
        S.emit(nc, final_wait_ops=outs)
    return nc


def prep_inputs(inp):
    f = lambda a: np.ascontiguousarray(np.asarray(a, dtype=np.float32))
    x = f(inp["x"])
    c = f(inp["c"])
    fm = lambda v: np.ascontiguousarray(v.reshape(-1, 128).T)
    pcols = np.zeros((128, 2 * NPL), np.float32)
    b_ada, ng, b_in = f(inp["b_ada"]), f(inp["norm_gain"]), f(inp["b_in"])
    conv_b, lcg, lcb, gg = f(inp["conv_b"]), f(inp["ln_conv_gain"]), f(inp["ln_conv_bias"]), f(inp["group_gain"])
    conv_w, b_sp = f(inp["conv_w"]), f(inp["b_spatial"])
    for l in range(2):
        o = l * NPL
        pcols[:, o:o + 48] = fm(b_ada[l])
        for i in range(4):
            pcols[:, o + 48 + 8 * i:o + 56 + 8 * i] = fm(ng[l, i])
        pcols[:, o + 80:o + 88] = fm(b_in[l, 1024:])
        pcols[:, o + 88:o + 92] = fm(conv_b[l])
        pcols[:, o + 92:o + 96] = fm(lcg[l])
        pcols[:, o + 96:o + 100] = fm(lcb[l])
        pcols[:, o + 100:o + 104] = fm(gg[l, 512:])
        cw = conv_w[l].reshape(31, 4, 128)
        pcols[:, o + 104:o + 228] = cw.transpose(2, 0, 1).reshape(128, 124)
        pcols[:, o + 228:o + 236] = b_sp[l].T
    rowvecs = np.stack([np.stack([f(inp["ln_v_gain"])[l], f(inp["ln_v_bias"])[l], gg[l, :512]]) for l in range(2)])
    wsT = np.ascontiguousarray(f(inp["w_spatial"]).transpose(0, 3, 1, 2))
    shared = {
        "pcols": pcols,
        "ident": np.eye(128, dtype=np.float32),
        "w_ada": f(inp["w_ada"]),
        "w_in": f(inp["w_in"]),
        "w_out": f(inp["w_out"]),
        "b_in_row": np.ascontiguousarray(b_in[:, :1024]),
        "rowvecs": np.ascontiguousarray(rowvecs),
        "wsT": wsT,
        "ffn_wg": f(inp["ffn_w_gate"]),
        "ffn_wu": f(inp["ffn_w_up"]),
        "ffn_wd": f(inp["ffn_w_down"]),
        "router_w": f(inp["router_w"])[0],
        "router_b": f(inp["router_b"])[0],
        "moe_wg": f(inp["moe_w_gate"])[0],
        "moe_wu": f(inp["moe_w_up"])[0],
        "moe_wd": f(inp["moe_w_down"])[0],
    }
    maps = []
    for b in range(8):
        m = dict(shared)
        m["xT"] = np.ascontiguousarray(x[b].T)
        m["cT"] = fm(c[b])
        maps.append(m)
    return maps


_NC_CACHE = {}


def kernel(**inputs):
    maps = prep_inputs(inputs)
    if "nc" not in _NC_CACHE:
        _NC_CACHE["nc"] = build_program()
    res = run_bass_kernel_spmd(_NC_CACHE["nc"], maps, core_ids=list(range(8)))
    out = np.stack([np.ascontiguousarray(r["outT"].T) for r in res.results]).astype(np.float32)
    return out
```

```python
from contextlib import ExitStack

import numpy as np
import concourse.bass as bass
import concourse.mybir as mybir
from concourse.bass_utils import run_bass_kernel_spmd

F32 = mybir.dt.float32
BF16 = mybir.dt.bfloat16
AF = mybir.ActivationFunctionType
ALU = mybir.AluOpType
AX = mybir.AxisListType

D = 1024
T = 2048
KC = 8
DFF = 3584
NFC = 28
NE = 8
EPS = 1e-6
MT = 256
NMT = T // MT
FT = 512
NFT = T // FT
NPL = 236
COMPUTE = ("pe", "act", "dve", "pool")


class Op:
    __slots__ = ("eng", "fn", "deps", "sig", "val", "semkey", "nosync", "gi")


class Sched:
    def __init__(self):
        self.ops = {e: [] for e in ("pe", "act", "dve", "pool", "sp")}
        self.lastw = {}
        self.readers = {}
        self.n = 0
        self.dmas = []

    def add(self, eng, fn, reads=(), writes=(), dma=None, nosync=False):
        op = Op()
        op.eng = eng
        op.fn = fn
        op.sig = False
        op.val = None
        op.semkey = dma
        op.nosync = nosync
        op.gi = self.n
        deps = {}
        for k in reads:
            w = self.lastw.get(k)
            if w is not None:
                deps[id(w)] = w
        for k in writes:
            w = self.lastw.get(k)
            if w is not None:
                deps[id(w)] = w
            rd = self.readers.get(k)
            if rd:
                for r in rd.values():
                    deps[id(r)] = r
        op.deps = list(deps.values())
        for k in writes:
            self.lastw[k] = op
            self.readers[k] = {}
        for k in reads:
            rk = eng if dma is None else ("dma", self.n)
            self.readers.setdefault(k, {})[rk] = op
        self.ops[eng].append(op)
        if dma is not None:
            self.dmas.append(op)
        self.n += 1
        return op

    def barrier(self):
        lasts = []
        for lst in self.ops.values():
            for op in reversed(lst):
                if op.semkey is None and op.fn is not None:
                    lasts.append(op)
                    break
        deps = lasts + self.dmas
        for e in self.ops:
            op = Op()
            op.eng = e
            op.fn = None
            op.sig = False
            op.val = None
            op.semkey = None
            op.nosync = False
            op.gi = self.n
            op.deps = [d for d in deps]
            self.ops[e].append(op)
        self.dmas = []
        self.lastw = {}
        self.readers = {}

    def emit(self, nc, final_wait_ops=()):
        for e, lst in self.ops.items():
            for op in lst:
                for d in op.deps:
                    if d.semkey is None:
                        if d.eng == op.eng and op.semkey is None and op.nosync:
                            continue
                        d.sig = True
        cnt = {e: 0 for e in COMPUTE}
        dcnt = {}
        for e, lst in self.ops.items():
            for op in lst:
                if op.semkey is not None:
                    dcnt[op.semkey] = dcnt.get(op.semkey, 0) + 16
                    op.val = dcnt[op.semkey]
                elif op.sig:
                    cnt[op.eng] += 1
                    op.val = cnt[op.eng]
        with ExitStack() as st:
            sems = {e: st.enter_context(nc.semaphore("s_" + e)) for e in COMPUTE}
            dsems = {k: st.enter_context(nc.semaphore("d_" + str(i))) for i, k in enumerate(dcnt)}
            block = st.enter_context(nc.Block())
            sched = self

            def needs_of(op):
                need = {}
                for d in op.deps:
                    if d.semkey is not None:
                        s = dsems[d.semkey]
                    else:
                        if not d.sig:
                            continue
                        if d.eng == op.eng and op.semkey is None and op.nosync:
                            continue
                        s = sems[d.eng]
                    cur = need.get(s.num)
                    if cur is None or cur[1] < d.val:
                        need[s.num] = (s, d.val, d.gi)
                return need

            def run(eng_name, eng):
                known = {}
                lst = sched.ops[eng_name]
                needs = [needs_of(op) for op in lst]
                look = 6 if eng_name == "pe" else 0
                for j, op in enumerate(lst):
                    for num, (s, v, _) in needs[j].items():
                        if known.get(num, 0) < v:
                            vv = v
                            for jj in range(j + 1, min(len(lst), j + 1 + look)):
                                nx = needs[jj].get(num)
                                if nx is not None and nx[2] < op.gi and nx[1] > vv:
                                    vv = nx[1]
                            eng.wait_ge(s, vv)
                            known[num] = vv
                    if op.fn is None:
                        continue
                    ins = op.fn(eng)
                    if op.semkey is not None:
                        ins.then_inc(dsems[op.semkey], 16)
                    elif op.sig:
                        ins.then_inc(sems[op.eng], 1)
                if eng_name == "sp":
                    for op in final_wait_ops:
                        eng.wait_ge(dsems[op.semkey], op.val)

            block.sync(lambda e: run("sp", e))
            block.scalar(lambda e: run("act", e))
            block.vector(lambda e: run("dve", e))
            block.gpsimd(lambda e: run("pool", e))
            block.tensor(lambda e: run("pe", e))


class Arena:
    def __init__(self, ap, nbytes):
        self.ap = ap
        self.nbytes = nbytes
        self.off = 0

    def reset(self):
        self.off = 0

    def alloc(self, free_shape, dtype):
        esz = 4 if dtype == F32 else 2
        n = int(np.prod(free_shape)) * esz
        n_al = (n + 31) // 32 * 32
        assert self.off + n_al <= self.nbytes, ("arena overflow", self.off, n_al, self.nbytes)
        v = self.ap[:, self.off // 2:(self.off + n) // 2]
        self.off += n_al
        if dtype == F32:
            v = v.bitcast(F32)
        if len(free_shape) == 2:
            v = v.rearrange("p (a b) -> p a b", b=free_shape[1])
        elif len(free_shape) == 3:
            v = v.rearrange("p (a b c) -> p a b c", b=free_shape[1], c=free_shape[2])
        return v


def build_program(n_layers=2, stop_after=None):
    nc = bass.Bass("TRN2", target_bir_lowering=False)

    def din(name, shape):
        return nc.dram_tensor(name, list(shape), F32, kind="ExternalInput").ap()

    xT_d = din("xT", [D, T])
    cT_d = din("cT", [128, KC])
    pcols_d = din("pcols", [128, 2 * NPL])
    ident_d = din("ident", [128, 128])
    w_ada_d = din("w_ada", [2, D, 6 * D])
    w_in_d = din("w_in", [2, D, 2 * D])
    w_out_d = din("w_out", [2, D, D])
    b_in_row_d = din("b_in_row", [2, D])
    rowvecs_d = din("rowvecs", [2, 3, 512])
    wsT_d = din("wsT", [2, 128, 8, 128])
    ffn_wg_d = din("ffn_wg", [1, D, DFF])
    ffn_wu_d = din("ffn_wu", [1, D, DFF])
    ffn_wd_d = din("ffn_wd", [1, DFF, D])
    rw_d = din("router_w", [D, NE])
    rb_d = din("router_b", [NE])
    moe_wg_d = din("moe_wg", [NE, D, DFF])
    moe_wu_d = din("moe_wu", [NE, D, DFF])
    moe_wd_d = din("moe_wd", [NE, DFF, D])
    out_d = nc.dram_tensor("outT", [D, T], F32, kind="ExternalOutput").ap()

    ARENA_BYTES = 132 * 1024
    with ExitStack() as st:
        def sb(name, shape, dt):
            return st.enter_context(nc.sbuf_tensor(name, list(shape), dt))

        xT = sb("xT_sb", [128, KC, T], F32)
        pcols = sb("pcols_sb", [128, 2 * NPL], F32)
        modt = sb("modt", [128, 2, 48], F32)
        dcols = sb("dcols", [128, 2, 32], F32)
        ident32 = sb("ident32", [128, 128], F32)
        identbf = sb("identbf", [128, 128], BF16)
        onesD = sb("onesD", [128, 128], BF16)
        ones512 = sb("ones512", [128, 128], BF16)
        ones32_512 = sb("ones32_512", [128, 128], F32)
        ones32 = sb("ones32", [128, 128], F32)
        onesrow = sb("onesrow", [1, 128], BF16)
        epsc = sb("epsc", [128, 1], F32)
        cwh = sb("cwh", [128, 124], F32)
        eps4 = sb("eps4", [128, 1], F32)
        cst = sb("cst", [128, KC], F32)
        cact = sb("cact", [128, KC], BF16)
        small = sb("small", [128, 64], F32)
        lg = sb("lg", [128, 16, NE], F32)
        lg2 = sb("lg2", [128, 16, NE], F32)
        eq1 = sb("eq1", [128, 16, NE], F32)
        eq2 = sb("eq2", [128, 16, NE], F32)
        gates = sb("gates", [128, 16, NE], F32)
        rt = sb("rt", [128, 6, 16], F32)
        rw32 = sb("rw32", [128, KC, NE], F32)
        rb_bc = sb("rb_bc", [128, NE], F32)
        arena_t = sb("arena", [128, ARENA_BYTES // 2], BF16)
        ps = st.enter_context(nc.psum_tensor("ps", [128, 8, 512], F32))
        A = Arena(arena_t[:], ARENA_BYTES)
        S = Sched()

        def ACT(out, in_, func, r, w, bias=None, scale=None, accum=None, nosync=False):
            kw = {}
            if bias is not None:
                kw["bias"] = bias
            if scale is not None:
                kw["scale"] = scale
            if accum is not None:
                kw["accum_out"] = accum
            return S.add("act", lambda e: e.activation(out=out, in_=in_, func=func, **kw), r, w, nosync=nosync)

        def TT(eng, out, in0, in1, op, r, w, nosync=False):
            return S.add(eng, lambda e: e.tensor_tensor(out=out, in0=in0, in1=in1, op=op), r, w, nosync=nosync)

        def STT(eng, out, in0, scalar, in1, op0, op1, r, w, nosync=False):
            return S.add(eng, lambda e: e.scalar_tensor_tensor(out=out, in0=in0, scalar=scalar, in1=in1, op0=op0, op1=op1), r, w, nosync=nosync)

        def TS(eng, out, in0, s1, s2, op0, op1, r, w, nosync=False):
            if s2 is None:
                return S.add(eng, lambda e: e.tensor_scalar(out=out, in0=in0, scalar1=s1, scalar2=None, op0=op0), r, w, nosync=nosync)
            return S.add(eng, lambda e: e.tensor_scalar(out=out, in0=in0, scalar1=s1, scalar2=s2, op0=op0, op1=op1), r, w, nosync=nosync)

        def MM(out, lhsT, rhs, start, stop, r, w):
            return S.add("pe", lambda e: e.matmul(out, lhsT, rhs, start=start, stop=stop), r, w, nosync=True)

        def DMA(eng, out, in_, r, w, key):
            return S.add(eng, lambda e: e.dma_start(out=out, in_=in_), r, w, dma=key)

        def MEMSET(eng, ap, val, w):
            return S.add(eng, lambda e: e.memset(ap, val), (), w)

        def rsqrt_tile(out_sb, in_ps, r, w, wkey_tmp):
            ACT(out_sb, in_ps, AF.Sqrt, r + ["epsc"], w, bias=epsc[:, 0:1])
            S.add("dve", lambda e: e.reciprocal(out=out_sb, in_=out_sb), w, w)

        DMA("sp", pcols[:], pcols_d, [], ["pcols"], "pcols")
        DMA("sp", cst[:], cT_d, [], ["cst"], "cst")
        DMA("sp", ident32[:], ident_d, [], ["ident32"], "ident")
        for k in range(KC):
            DMA("sp", xT[:, k, :], xT_d[k * 128:(k + 1) * 128, :], [], [("x", k, mt) for mt in range(NMT)], ("xin", k))
        MEMSET("dve", onesD[:], 1.0 / D, ["onesD"])
        MEMSET("dve", ones512[:], 1.0 / 512, ["ones512"])
        MEMSET("dve", ones32_512[:], 1.0 / 512, ["ones32_512"])
        MEMSET("dve", ones32[:], 1.0, ["ones32"])
        MEMSET("dve", onesrow[:], 1.0, ["onesrow"])
        MEMSET("dve", epsc[:], EPS, ["epsc"])
        MEMSET("dve", eps4[:], 4.0 * EPS, ["eps4"])
        ACT(identbf[:], ident32[:], AF.Copy, ["ident32"], ["identbf"])
        ACT(cact[:], cst[:], AF.Silu, ["cst"], ["cact"])

        def pc(l, c0, n=1):
            return pcols[:, l * NPL + c0:l * NPL + c0 + n]

        def mod_dma(l, pi, stage):
            ns = len(stage)
            sl = stage[pi % ns]
            DMA("pool", sl, w_ada_d[l, :, pi * 256:(pi + 1) * 256].rearrange("(k p) n -> p k n", p=128), [], [("wada", pi % ns)], ("wada", pi % ns))

        def mod_mm(l, pi, stage):
            ns = len(stage)
            sl = stage[pi % ns]
            for o in range(2):
                oc = pi * 2 + o
                for k in range(KC):
                    MM(ps[:, 5, 256 + oc:256 + oc + 1], sl[:, k, o * 128:(o + 1) * 128], cact[:, k:k + 1], k == 0, k == KC - 1,
                       [("wada", pi % ns), "cact"], ["psmod"])

        def mod_fin(l):
            TT("dve", modt[:, l, :], ps[:, 5, 256:304], pc(l, 0, 48), ALU.add, ["psmod", "pcols"], [("mod", l)])
            STT("dve", dcols[:, l, 0:8], modt[:, l, 8:16], 1.0, pc(l, 48, 8), ALU.add, ALU.mult, [("mod", l), "pcols"], [("dc", l, 0)])
            TT("dve", dcols[:, l, 8:16], modt[:, l, 16:24], pc(l, 56, 8), ALU.mult, [("mod", l), "pcols"], [("dc", l, 1)])
            STT("dve", dcols[:, l, 16:24], modt[:, l, 32:40], 1.0, pc(l, 64, 8), ALU.add, ALU.mult, [("mod", l), "pcols"], [("dc", l, 2)])
            TT("dve", dcols[:, l, 24:32], modt[:, l, 40:48], pc(l, 72, 8), ALU.mult, [("mod", l), "pcols"], [("dc", l, 3)])

        def norm_mod_tile(l, which, c0, ncol, sq_bufs, rstd_buf, tmp_bufs, out_fn, xkeys_fn, stat_bank, h32_bufs=None, post=None):
            gsc = 0 if which == 1 else 16
            shc = 0 if which == 1 else 24
            for k in range(KC):
                sq = sq_bufs[k % 2]
                ACT(sq, xT[:, k, c0:c0 + ncol], AF.Square, [xkeys_fn(k)], [("sqA", k % 2)])
                MM(ps[:, stat_bank, 0:ncol], onesD[:], sq, k == 0, k == KC - 1, [("sqA", k % 2), "onesD"], [("ps", stat_bank)])
            rsqrt_tile(rstd_buf, ps[:, stat_bank, 0:ncol], [("ps", stat_bank)], ["rstdA"], "rstdA_t")
            for k in range(KC):
                tmp = tmp_bufs[k % 2]
                TT("dve", tmp, xT[:, k, c0:c0 + ncol], rstd_buf, ALU.mult, [xkeys_fn(k), "rstdA"], [("tmpA", k % 2)])
                dst, dkeys = out_fn(k)
                if h32_bufs is None:
                    ACT(dst, tmp, AF.Identity, [("tmpA", k % 2), ("dc", l, 0 if which == 1 else 2), ("mod", l)], dkeys,
                        scale=dcols[:, l, gsc + k:gsc + k + 1], bias=modt[:, l, shc + k:shc + k + 1])
                else:
                    h32 = h32_bufs[k % 2]
                    ACT(h32, tmp, AF.Identity, [("tmpA", k % 2), ("dc", l, 2), ("mod", l)], [("h32", k % 2)],
                        scale=dcols[:, l, gsc + k:gsc + k + 1], bias=modt[:, l, shc + k:shc + k + 1])
                    S.add("pool", lambda e, dst=dst, h32=h32: e.tensor_copy(out=dst, in_=h32), [("h32", k % 2)], dkeys)
                    post(k, h32, ("h32", k % 2))

        def mixer(l, do_mod_next):
            A.reset()
            w_in = A.alloc([KC, 2 * D], BF16)
            w_out = A.alloc([KC, D], BF16)
            hb = A.alloc([KC, MT], BF16)
            ycat = [A.alloc([KC, MT], BF16) for _ in range(3)]
            wsT = A.alloc([8, 128], BF16)
            bct = A.alloc([3, 512], F32)
            binrow = A.alloc([D], BF16)
            ub = [A.alloc([512], F32) for _ in range(2)]
            gv = A.alloc([512], F32)
            vb = [A.alloc([512], BF16) for _ in range(2)]
            yanb = [A.alloc([512], BF16) for _ in range(2)]
            junk = A.alloc([512], BF16)
            sg = A.alloc([MT], F32)
            abb = junk.bitcast(F32)
            xg = [A.alloc([4, 30 + MT], F32) for _ in range(2)]
            ycb = [A.alloc([4, MT], F32) for _ in range(2)]
            sqc = [A.alloc([MT], F32) for _ in range(2)]
            mean_sb = A.alloc([MT], F32)
            m2 = A.alloc([MT], F32)
            sqA = [A.alloc([MT], BF16) for _ in range(2)]
            sqB = [A.alloc([MT], BF16) for _ in range(2)]
            rstdA = A.alloc([MT], F32)
            tmpA = [A.alloc([MT], F32) for _ in range(2)]
            yo = A.alloc([KC, MT], F32)
            sqF = [A.alloc([MT], BF16) for _ in range(2)]
            rsF = A.alloc([MT], F32)
            tmpF = [A.alloc([MT], F32) for _ in range(2)]
            stage = [A.alloc([KC, 256], BF16) for _ in range(2)]

            DMA("pool", w_in, w_in_d[l].rearrange("(k p) n -> p k n", p=128), [], ["w_in"], "w_in")
            DMA("pool", wsT, wsT_d[l], [], ["wsT_raw"], "wsT")
            DMA("pool", binrow[0:1, :], b_in_row_d[l:l + 1, :], [], ["binrow"], "binrow")
            DMA("sp", bct, rowvecs_d[l].partition_broadcast(128), [], ["bct"], "bct")
            DMA("pool", w_out, w_out_d[l].rearrange("(k p) n -> p k n", p=128), [], ["w_out"], "w_out")
            MEMSET("dve", wsT[64:128, :, 0:64], 0.0, ["wsT_raw"])
            MEMSET("dve", xg[0][:, :, 0:30], 0.0, [("xg_halo", 0)])
            TS("dve", small[:, 32:36], pc(l, 84, 4), 0.5, None, ALU.mult, None, ["pcols"], ["hbcol"])
            TS("dve", cwh[:], pc(l, 104, 124), 0.5, None, ALU.mult, None, ["pcols"], ["cwh"])
            zi = [0]
            fi = [0]

            def zbank():
                zi[0] += 1
                return zi[0] % 2

            def fbank():
                fi[0] += 1
                return 2 + fi[0] % 2

            NB = MT // 128

            def rsq(out_sb, in_ap, r, w, e4=False):
                ACT(out_sb, in_ap, AF.Sqrt, r + ["epsc", "eps4"], w, bias=(eps4 if e4 else epsc)[:, 0:1])
                S.add("dve", lambda e: e.reciprocal(out=out_sb, in_=out_sb), w, w)

            def front(mt):
                c0 = mt * MT
                yb = ycat[mt % 3]
                xgb = xg[mt % 2]
                hk = lambda k: ("hT", k)
                if do_mod_next:
                    mod_dma(l + 1, 3 * mt, stage)
                    mod_dma(l + 1, 3 * mt + 1, stage)
                for k in range(KC):
                    sq = sqA[k % 2]
                    ACT(sq, xT[:, k, c0:c0 + MT], AF.Square, [("x", k, mt)], [("sqA", k % 2)])
                    MM(ps[:, 4, 0:MT], onesD[:], sq, k == 0, k == KC - 1, [("sqA", k % 2), "onesD"], [("ps", 4)])
                    yield
                rsq(rstdA, ps[:, 4, 0:MT], [("ps", 4)], ["rstdA"])
                yield
                for k in range(KC):
                    tmp = tmpA[k % 2]
                    TT("dve", tmp, xT[:, k, c0:c0 + MT], rstdA, ALU.mult, [("x", k, mt), "rstdA"], [("tmpA", k % 2)])
                    ACT(hb[:, k, :], tmp, AF.Identity, [("tmpA", k % 2), ("dc", l, 0), ("mod", l)], [hk(k)],
                        scale=dcols[:, l, k:k + 1], bias=modt[:, l, k:k + 1])
                    yield
                for bi in range(NB):
                    tb = mt * NB + bi
                    b0 = bi * 128
                    pb = tb % 2
                    u = ub[pb]
                    v = vb[pb]
                    yan = yanb[pb]
                    bu = zbank()
                    for k in range(KC):
                        MM(ps[:, bu, :], hb[:, k, b0:b0 + 128], w_in[:, k, 0:512], k == 0, False, [hk(k), "w_in"], [("ps", bu)])
                    MM(ps[:, bu, :], onesrow[0:1, :], binrow[0:1, 0:512], False, True, ["onesrow", "binrow"], [("ps", bu)])
                    yield
                    bv = zbank()
                    for k in range(KC):
                        MM(ps[:, bv, :], hb[:, k, b0:b0 + 128], w_in[:, k, 512:1024], k == 0, False, [hk(k), "w_in"], [("ps", bv)])
                    MM(ps[:, bv, :], onesrow[0:1, :], binrow[0:1, 512:1024], False, True, ["onesrow", "binrow"], [("ps", bv)])
                    yield
                    ACT(u, ps[:, bu, :], AF.Gelu_apprx_tanh, [("ps", bu)], [("u", pb)])
                    yield
                    ACT(gv, ps[:, bv, :], AF.Gelu_apprx_tanh, [("ps", bv)], ["gv"])
                    yield
                    s0 = pb * 16
                    S.add("dve", lambda e, s0=s0: e.bn_stats(out=small[:, s0:s0 + 6], in_=gv), ["gv"], [("st6", pb)])
                    S.add("dve", lambda e, s0=s0: e.bn_aggr(out=small[:, s0 + 6:s0 + 8], in_=small[:, s0:s0 + 6]), [("st6", pb)], [("mv", pb)])
                    yield
                    ACT(small[:, s0 + 8:s0 + 9], small[:, s0 + 7:s0 + 8], AF.Sqrt, [("mv", pb), "epsc"], [("sd", pb)], bias=epsc[:, 0:1])
                    S.add("dve", lambda e, s0=s0: e.reciprocal(out=small[:, s0 + 9:s0 + 10], in_=small[:, s0 + 8:s0 + 9]), [("sd", pb)], [("rv", pb)])
                    yield
                    STT("dve", small[:, s0 + 10:s0 + 11], small[:, s0 + 6:s0 + 7], -1.0, small[:, s0 + 9:s0 + 10], ALU.mult, ALU.mult,
                        [("mv", pb), ("rv", pb)], [("nb", pb)])
                    yield
                    ACT(gv, gv, AF.Identity, ["gv", ("rv", pb), ("nb", pb)], ["gv"],
                        scale=small[:, s0 + 9:s0 + 10], bias=small[:, s0 + 10:s0 + 11])
                    yield
                    TT("dve", gv, gv, bct[:, 0, :], ALU.mult, ["gv", "bct"], ["gv"])
                    yield
                    TT("dve", v, gv, bct[:, 1, :], ALU.add, ["gv", "bct"], [("v", pb)])
                    yield
                    bm = 4
                    for h in range(8):
                        MM(ps[:, bm, h * 64:(h + 1) * 64], wsT[:, h, :], v[:, h * 64:(h + 1) * 64], True, True,
                           ["wsT_raw", ("v", pb)], [("ps", bm)])
                    yield
                    for h in range(8):
                        STT("dve", u[:, h * 64:(h + 1) * 64], ps[:, bm, h * 64:(h + 1) * 64], pc(l, 228 + h), u[:, h * 64:(h + 1) * 64],
                            ALU.add, ALU.mult, [("ps", bm), ("u", pb), "pcols"], [("u", pb)], nosync=(h > 0))
                        if h % 2 == 1:
                            yield
                    MEMSET("dve", small[:, s0 + 11:s0 + 12], 0.0, [("ssA", pb)])
                    ACT(junk, u, AF.Square, [("u", pb), ("ssA", pb)], ["junk", ("ssA", pb)], accum=small[:, s0 + 11:s0 + 12])
                    yield
                    TS("dve", small[:, s0 + 12:s0 + 13], small[:, s0 + 11:s0 + 12], 1.0 / 512, EPS, ALU.mult, ALU.add, [("ssA", pb)], [("ssA2", pb)])
                    ACT(small[:, s0 + 13:s0 + 14], small[:, s0 + 12:s0 + 13], AF.Sqrt, [("ssA2", pb)], [("sdA", pb)])
                    S.add("dve", lambda e, s0=s0: e.reciprocal(out=small[:, s0 + 14:s0 + 15], in_=small[:, s0 + 13:s0 + 14]), [("sdA", pb)], [("rsA", pb)])
                    yield
                    STT("dve", yan, u, small[:, s0 + 14:s0 + 15], bct[:, 2, :], ALU.mult, ALU.mult, [("u", pb), ("rsA", pb), "bct"], [("yan", pb)])
                    yield
                    psT = ps[:, 5, 0:256].bitcast(BF16).rearrange("p (j t) -> p j t", t=128)
                    for j in range(4):
                        S.add("pe", lambda e, j=j, yan=yan, psT=psT: e.transpose(out=psT[:, j, :], in_=yan[:, j * 128:(j + 1) * 128], identity=identbf[:]),
                              [("yan", pb), "identbf"], [("ps", 5)], nosync=True)
                    ACT(yb[:, 0:4, b0:b0 + 128], psT, AF.Copy, [("ps", 5)], [("yc", mt % 3, j, bi) for j in range(4)])
                    yield
                if mt > 0:
                    S.add("pool", lambda e, xgb=xgb, prev=xg[(mt - 1) % 2]: e.tensor_copy(out=xgb[:, :, 0:30], in_=prev[:, :, MT:MT + 30]),
                          [("xg", (mt - 1) % 2, j) for j in range(4)], [("xg_halo", mt % 2)])
                for j in range(4):
                    ba = zbank()
                    for k in range(KC):
                        MM(ps[:, ba, 0:MT], w_in[:, k, (8 + j) * 128:(9 + j) * 128], hb[:, k, :], k == 0, k == KC - 1, [hk(k), "w_in"], [("ps", ba)])
                    yield
                    bg = zbank()
                    for k in range(KC):
                        MM(ps[:, bg, 0:MT], w_in[:, k, (12 + j) * 128:(13 + j) * 128], hb[:, k, :], k == 0, k == KC - 1, [hk(k), "w_in"], [("ps", bg)])
                    yield
                    ACT(sg, ps[:, bg, 0:MT], AF.Tanh, [("ps", bg), "hbcol"], ["sg"], scale=0.5, bias=small[:, 32 + j:33 + j])
                    ACT(abb, ps[:, ba, 0:MT], AF.Identity, [("ps", ba), "pcols"], ["junk"], bias=pc(l, 80 + j))
                    yield
                    STT("dve", xgb[:, j, 30:30 + MT], sg, 1.0, abb, ALU.add, ALU.mult,
                        ["junk", "sg"], [("xg", mt % 2, j)])
                    yield
                if do_mod_next:
                    mod_mm(l + 1, 3 * mt, stage)
                    yield
                    mod_dma(l + 1, 3 * mt + 2, stage)
                    mod_mm(l + 1, 3 * mt + 1, stage)
                    yield
                    mod_mm(l + 1, 3 * mt + 2, stage)
                    yield

            def conv(mt):
                xgb = xg[mt % 2]
                yc = ycb[mt % 2]
                xr = lambda j: [("xg", mt % 2, j), ("xg_halo", mt % 2)]
                for j in range(4):
                    TS("dve", yc[:, j, :], xgb[:, j, 0:MT], cwh[:, j:j + 1], pc(l, 88 + j), ALU.mult, ALU.add,
                       xr(j) + ["pcols", "cwh"], [("ycv", mt % 2, j)])
                    yield
                    for tap in range(1, 31):
                        STT("dve", yc[:, j, :], xgb[:, j, tap:tap + MT], cwh[:, tap * 4 + j:tap * 4 + j + 1], yc[:, j, :], ALU.mult, ALU.add,
                            xr(j) + [("ycv", mt % 2, j)], [("ycv", mt % 2, j)], nosync=True)
                        if tap % 2 == 0:
                            yield

            def post(mt):
                c0 = mt * MT
                yb = ycat[mt % 3]
                yc = ycb[mt % 2]
                yk = lambda j: ("ycv", mt % 2, j)
                for j in range(4):
                    MM(ps[:, 6, 0:MT], ones32_512[:], yc[:, j, :], j == 0, j == 3, [yk(j), "ones32_512"], [("ps", 6)])
                yield
                for j in range(4):
                    ACT(sqc[j % 2], yc[:, j, :], AF.Square, [yk(j)], [("sqc", j % 2)])
                    MM(ps[:, 7, 0:MT], ones32_512[:], sqc[j % 2], j == 0, j == 3, [("sqc", j % 2), "ones32_512"], [("ps", 7)])
                    yield
                ACT(mean_sb, ps[:, 6, 0:MT], AF.Copy, [("ps", 6)], ["mean_sb"])
                yield
                TT("dve", m2, mean_sb, mean_sb, ALU.mult, ["mean_sb"], ["m2"])
                yield
                TT("dve", m2, ps[:, 7, 0:MT], m2, ALU.subtract, [("ps", 7), "m2"], ["m2"])
                yield
                rsq(m2, m2, ["m2"], ["m2"])
                yield
                for j in range(4):
                    TT("pool", yc[:, j, :], yc[:, j, :], mean_sb, ALU.subtract, [yk(j), "mean_sb"], [yk(j)])
                    yield
                    TT("pool", yc[:, j, :], yc[:, j, :], m2, ALU.mult, [yk(j), "m2"], [yk(j)])
                    yield
                    ACT(yc[:, j, :], yc[:, j, :], AF.Identity, [yk(j), "pcols"], [yk(j)], scale=pc(l, 92 + j), bias=pc(l, 96 + j))
                    yield
                    ACT(sqc[j % 2], yc[:, j, :], AF.Tanh, [yk(j)], [("sqc", j % 2)], scale=0.5)
                    yield
                    STT("dve", yc[:, j, :], sqc[j % 2], 1.0, yc[:, j, :], ALU.add, ALU.mult, [yk(j), ("sqc", j % 2)], [yk(j)])
                    yield
                    ACT(sqB[j % 2], yc[:, j, :], AF.Square, [yk(j)], [("sqB", j % 2)])
                    MM(ps[:, 6, 0:MT], ones512[:], sqB[j % 2], j == 0, j == 3, [("sqB", j % 2), "ones512"], [("ps", 6)])
                    yield
                rsq(m2, ps[:, 6, 0:MT], [("ps", 6)], ["m2"], e4=True)
                yield
                for j in range(4):
                    TT("dve", yc[:, j, :], yc[:, j, :], m2, ALU.mult, [yk(j), "m2"], [yk(j)])
                    ACT(yb[:, 4 + j, :], yc[:, j, :], AF.Identity, [yk(j), "pcols"], [("yc", mt % 3, 4 + j, bi) for bi in range(NB)],
                        scale=pc(l, 100 + j))
                    yield
                ykeys = [[("yc", mt % 3, k, bi) for bi in range(NB)] for k in range(KC)]
                for dc in range(KC):
                    bo = fbank()
                    for k in range(KC):
                        MM(ps[:, bo, 0:MT], w_out[:, k, dc * 128:(dc + 1) * 128], yb[:, k, :], k == 0, k == KC - 1, ykeys[k] + ["w_out"], [("ps", bo)])
                    yield
                    ACT(yo[:, dc, :], ps[:, bo, 0:MT], AF.Copy, [("ps", bo)], [("yo", dc)])
                    ACT(sqF[dc % 2], ps[:, bo, 0:MT], AF.Square, [("ps", bo)], [("sqF", dc % 2)])
                    MM(ps[:, 7, 0:MT], onesD[:], sqF[dc % 2], dc == 0, dc == KC - 1, [("sqF", dc % 2), "onesD"], [("ps", 7)])
                    yield
                rsq(rsF, ps[:, 7, 0:MT], [("ps", 7)], ["rsF"])
                yield
                for dc in range(KC):
                    STT("dve", tmpF[dc % 2], yo[:, dc, :], dcols[:, l, 8 + dc:9 + dc], rsF, ALU.mult, ALU.mult,
                        [("yo", dc), "rsF", ("dc", l, 1)], [("tmpF", dc % 2)])
                    TT("pool", xT[:, dc, c0:c0 + MT], xT[:, dc, c0:c0 + MT], tmpF[dc % 2], ALU.add, [("x", dc, mt), ("tmpF", dc % 2)], [("x", dc, mt)])
                    yield

            def drive(gens):
                gens = [g for g in gens if g is not None]
                while gens:
                    for g in list(gens):
                        try:
                            next(g)
                        except StopIteration:
                            gens.remove(g)

            drive([front(0)])
            drive([conv(0), front(1)])
            for mt in range(1, NMT):
                drive([conv(mt), front(mt + 1) if mt + 1 < NMT else None, post(mt - 1)])
            drive([post(NMT - 1)])
            if do_mod_next:
                mod_fin(l + 1)
            S.barrier()

        def ffn(l, moe):
            A.reset()
            hT = A.alloc([KC, T], BF16)
            acc = A.alloc([KC, T], F32)
            acc_off = 32768
            wg = [A.alloc([KC, 256], BF16) for _ in range(2)]
            wu = [A.alloc([KC, 256], BF16) for _ in range(2)]
            wd = [A.alloc([2, D], BF16) for _ in range(2)]
            wslot_off = 32768 + 65536
            asl = [[A.alloc([FT], BF16) for _ in range(2)] for _ in range(2)]
            gate_bc = A.alloc([T], BF16)
            tsl = [A.alloc([FT], BF16) for _ in range(2)]
            dg = A.alloc([4, 128], F32)
            A2 = Arena(arena_t[:], acc_off + 65536)
            A2.off = acc_off
            sqA = [A2.alloc([FT], BF16) for _ in range(2)]
            rstdA = A2.alloc([FT], F32)
            tmpA = [A2.alloc([FT], F32) for _ in range(2)]
            h32b = [A2.alloc([FT], F32) for _ in range(2)]
            AG = Arena(arena_t[:], wslot_off + 24576)
            AG.off = wslot_off
            sqG = [AG.alloc([FT], BF16) for _ in range(2)]
            rsG = AG.alloc([FT], F32)
            tmpG = [AG.alloc([FT], F32) for _ in range(2)]

            if moe:
                DMA("sp", rw32[:], rw_d.rearrange("(k p) e -> p k e", p=128), [], ["rw32"], "rw32")
                DMA("sp", rb_bc[:], rb_d.partition_broadcast(128), [], ["rb_bc"], "rb_bc")
            for ft in range(NFT):
                c0 = ft * FT
                xk = lambda k, ft=ft: ("x", k, ft)

                def post(k, h32, hkey, ft=ft):
                    for b in range(4):
                        MM(ps[:, b, 0:8], h32[:, b * 128:(b + 1) * 128], rw32[:, k, :], k == 0, k == KC - 1, [hkey, "rw32"], [("ps", b)])

                norm_mod_tile(l, 2, c0, FT, sqA, rstdA, tmpA,
                              lambda k, ft=ft: (hT[:, k, ft * FT:(ft + 1) * FT], [("hT", k, ft)]),
                              xk, 6, h32_bufs=h32b if moe else None, post=post if moe else None)
                if moe:
                    for b in range(4):
                        TT("dve", lg[:, ft * 4 + b, :], ps[:, b, 0:8], rb_bc[:], ALU.add, [("ps", b), "rb_bc"], [("lg", ft * 4 + b)])
            if moe:
                lgk = [("lg", i) for i in range(16)]
                bc3 = lambda ap: ap.unsqueeze(2).to_broadcast([128, 16, NE])
                S.add("dve", lambda e: e.tensor_reduce(out=rt[:, 0, :], in_=lg[:], axis=AX.X, op=ALU.max), lgk, ["m1"])
                TT("dve", eq1[:], lg[:], bc3(rt[:, 0, :]), ALU.is_equal, lgk + ["m1"], ["eq1"])
                STT("dve", lg2[:], eq1[:], -1e30, lg[:], ALU.mult, ALU.add, ["eq1"] + lgk, ["lg2"])
                S.add("dve", lambda e: e.tensor_reduce(out=rt[:, 1, :], in_=lg2[:], axis=AX.X, op=ALU.max), ["lg2"], ["m2r"])
                TT("dve", eq2[:], lg2[:], bc3(rt[:, 1, :]), ALU.is_equal, ["lg2", "m2r"], ["eq2"])
                TT("dve", rt[:, 2, :], rt[:, 1, :], rt[:, 0, :], ALU.subtract, ["m1", "m2r"], ["dm"])
                ACT(rt[:, 3, :], rt[:, 2, :], AF.Sigmoid, ["dm"], ["w1"], scale=-1.0)
                ACT(rt[:, 4, :], rt[:, 2, :], AF.Sigmoid, ["dm"], ["w2"])
                TT("dve", eq1[:], eq1[:], bc3(rt[:, 3, :]), ALU.mult, ["eq1", "w1"], ["eq1"])
                TT("dve", eq2[:], eq2[:], bc3(rt[:, 4, :]), ALU.mult, ["eq2", "w2"], ["eq2"])
                TT("dve", gates[:], eq1[:], eq2[:], ALU.add, ["eq1", "eq2"], ["gates"])
            S.barrier()

            experts = list(range(NE)) if moe else [0]
            wgd = moe_wg_d if moe else ffn_wg_d
            wud = moe_wu_d if moe else ffn_wu_d
            wdd = moe_wd_d if moe else ffn_wd_d
            steps = []
            gi = 0
            for e_ in experts:
                for g in range(NFC // 2):
                    for ft in range(NFT):
                        steps.append((e_, g, ft, gi))
                    gi += 1
            dbank = [0]
            first_acc = {}

            def down(step):
                e_, g, ft, gidx = step
                s = gidx % 2
                aslot = asl[step_index[step] % 2]
                for dc in range(KC):
                    b = 4 + dbank[0] % 4
                    dbank[0] += 1
                    MM(ps[:, b, :], wd[s][:, 0, dc * 128:(dc + 1) * 128], aslot[0], True, False, [("wd", s), ("a", step_index[step] % 2, 0)], [("ps", b)])
                    MM(ps[:, b, :], wd[s][:, 1, dc * 128:(dc + 1) * 128], aslot[1], False, True, [("wd", s), ("a", step_index[step] % 2, 1)], [("ps", b)])
                    dst = acc[:, dc, ft * FT:(ft + 1) * FT]
                    if (dc, ft) not in first_acc:
                        first_acc[(dc, ft)] = True
                        ACT(dst, ps[:, b, :], AF.Copy, [("ps", b)], [("acc", dc, ft)])
                    else:
                        TT("dve", dst, dst, ps[:, b, :], ALU.add, [("ps", b), ("acc", dc, ft)], [("acc", dc, ft)])
                    yield

            def load_group(step):
                e2, g2, _, gidx2 = step
                s2 = gidx2 % 2
                f0 = 2 * g2 * 128
                DMA("pool", wg[s2], wgd[e2, :, f0:f0 + 256].rearrange("(k p) n -> p k n", p=128), [], [("wg", s2)], ("wg", s2))
                DMA("pool", wu[s2], wud[e2, :, f0:f0 + 256].rearrange("(k p) n -> p k n", p=128), [], [("wu", s2)], ("wu", s2))
                DMA("pool", wd[s2], wdd[e2, f0:f0 + 256, :].rearrange("(f p) n -> p f n", p=128), [], [("wd", s2)], ("wd", s2))

            step_index = {st_: i for i, st_ in enumerate(steps)}
            prev = None
            cur_e = None
            for si, step in enumerate(steps):
                e_, g, ft, gidx = step
                s = gidx % 2
                if si == 0:
                    load_group(step)
                if moe and e_ != cur_e:
                    cur_e = e_
                    for t4 in range(NFT):
                        for b in range(4):
                            TS("dve", dg[:, b, :], ident32[:], gates[:, t4 * 4 + b, e_:e_ + 1], None, ALU.mult, None, ["gates", "ident32"], [("dg", b)])
                        MM(ps[:, 7, :], ones32[:], dg.rearrange("p a b -> p (a b)"), True, True, [("dg", b) for b in range(4)] + ["ones32"], [("ps", 7)])
                        ACT(gate_bc[:, t4 * FT:(t4 + 1) * FT], ps[:, 7, :], AF.Copy, [("ps", 7)], [("gbc", t4)])
                ai = si % 2
                dgen = down(prev) if prev is not None else iter(())

                def dn(n):
                    for _ in range(n):
                        next(dgen, None)

                for f in range(2):
                    bg_, bu_ = 2 * f, 2 * f + 1
                    for k in range(KC):
                        MM(ps[:, bg_, :], wg[s][:, k, f * 128:(f + 1) * 128], hT[:, k, ft * FT:(ft + 1) * FT], k == 0, k == KC - 1,
                           [("wg", s), ("hT", k, ft)], [("ps", bg_)])
                    ACT(tsl[f], ps[:, bg_, :], AF.Silu, [("ps", bg_)], [("ts", f)])
                    if f == 1:
                        dn(2)
                    for k in range(KC):
                        MM(ps[:, bu_, :], wu[s][:, k, f * 128:(f + 1) * 128], hT[:, k, ft * FT:(ft + 1) * FT], k == 0, k == KC - 1,
                           [("wu", s), ("hT", k, ft)], [("ps", bu_)])
                    if moe:
                        TT("dve", tsl[f], tsl[f], ps[:, bu_, :], ALU.mult, [("ts", f), ("ps", bu_)], [("ts", f)])
                        TT("pool", asl[ai][f], tsl[f], gate_bc[:, ft * FT:(ft + 1) * FT], ALU.mult, [("ts", f), ("gbc", ft)], [("a", ai, f)])
                    else:
                        TT("dve", asl[ai][f], tsl[f], ps[:, bu_, :], ALU.mult, [("ts", f), ("ps", bu_)], [("a", ai, f)])
                    dn(2)
                dn(8)
                prev = step
                if ft == 0 and si + NFT < len(steps):
                    load_group(steps[si + NFT])
            for _ in down(prev):
                pass
            S.barrier()
            for ft in range(NFT):
                c0 = ft * FT
                for dc in range(KC):
                    ACT(sqG[dc % 2], acc[:, dc, c0:c0 + FT], AF.Square, [("acc", dc, ft)], [("sqG", dc % 2)])
                    MM(ps[:, 6, :], onesD[:], sqG[dc % 2], dc == 0, dc == KC - 1, [("sqG", dc % 2), "onesD"], [("ps", 6)])
                rsqrt_tile(rsG, ps[:, 6, :], [("ps", 6)], ["rsG"], "rsG_t")
                for dc in range(KC):
                    STT("dve", tmpG[dc % 2], acc[:, dc, c0:c0 + FT], dcols[:, l, 24 + dc:25 + dc], rsG, ALU.mult, ALU.mult,
                        [("acc", dc, ft), "rsG", ("dc", l, 3)], [("tmpG", dc % 2)])
                    TT("dve", xT[:, dc, c0:c0 + FT], xT[:, dc, c0:c0 + FT], tmpG[dc % 2], ALU.add, [("x", dc, ft), ("tmpG", dc % 2)], [("x", dc, ft)])
            S.barrier()

        A.reset()
        stage0 = [A.alloc([KC, 256], BF16) for _ in range(3)]
        for pi in range(24):
            mod_dma(0, pi, stage0)
            mod_mm(0, pi, stage0)
        mod_fin(0)
        S.barrier()
        done = False
        for l in range(n_layers):
            mixer(l, do_mod_next=(l + 1 < n_layers))
            if stop_after == ("mixer", l):
                break
            ffn(l, moe=(l % 2 == 1))
            if stop_after == ("ffn", l):
                break
        outs = []
        for k in range(KC):
            outs.append(DMA("sp", out_d[k * 128:(k + 1) * 128, :], xT[:, k, :], [("x", k, i) for i in range(NMT)] + [("x", k, i) for i in range(NFT)], [], ("out", k)))
        S.emit(nc, final_wait_ops=outs)
    return nc


def prep_inputs(inp):
    f = lambda a: np.ascontiguousarray(np.asarray(a, dtype=np.float32))
    x = f(inp["x"])
    c = f(inp["c"])
    fm = lambda v: np.ascontiguousarray(v.reshape(-1, 128).T)
    pcols = np.zeros((128, 2 * NPL), np.float32)
    b_ada, ng, b_in = f(inp["b_ada"]), f(inp["norm_gain"]), f(inp["b_in"])
    conv_b, lcg, lcb, gg = f(inp["conv_b"]), f(inp["ln_conv_gain"]), f(inp["ln_conv_bias"]), f(inp["group_gain"])
    conv_w, b_sp = f(inp["conv_w"]), f(inp["b_spatial"])
    for l in range(2):
        o = l * NPL
        pcols[:, o:o + 48] = fm(b_ada[l])
        for i in range(4):
            pcols[:, o + 48 + 8 * i:o + 56 + 8 * i] = fm(ng[l, i])
        pcols[:, o + 80:o + 88] = fm(b_in[l, 1024:])
        pcols[:, o + 88:o + 92] = fm(conv_b[l])
        pcols[:, o + 92:o + 96] = fm(lcg[l])
        pcols[:, o + 96:o + 100] = fm(lcb[l])
        pcols[:, o + 100:o + 104] = fm(gg[l, 512:])
        cw = conv_w[l].reshape(31, 4, 128)
        pcols[:, o + 104:o + 228] = cw.transpose(2, 0, 1).reshape(128, 124)
        pcols[:, o + 228:o + 236] = b_sp[l].T
    rowvecs = np.stack([np.stack([f(inp["ln_v_gain"])[l], f(inp["ln_v_bias"])[l], gg[l, :512]]) for l in range(2)])
    wsT = np.ascontiguousarray(f(inp["w_spatial"]).transpose(0, 3, 1, 2))
    shared = {
        "pcols": pcols,
        "ident": np.eye(128, dtype=np.float32),
        "w_ada": f(inp["w_ada"]),
        "w_in": f(inp["w_in"]),
        "w_out": f(inp["w_out"]),
        "b_in_row": np.ascontiguousarray(b_in[:, :1024]),
        "rowvecs": np.ascontiguousarray(rowvecs),
        "wsT": wsT,
        "ffn_wg": f(inp["ffn_w_gate"]),
        "ffn_wu": f(inp["ffn_w_up"]),
        "ffn_wd": f(inp["ffn_w_down"]),
        "router_w": f(inp["router_w"])[0],
        "router_b": f(inp["router_b"])[0],
        "moe_wg": f(inp["moe_w_gate"])[0],
        "moe_wu": f(inp["moe_w_up"])[0],
        "moe_wd": f(inp["moe_w_down"])[0],
    }
    maps = []
    for b in range(8):
        m = dict(shared)
        m["xT"] = np.ascontiguousarray(x[b].T)
        m["cT"] = fm(c[b])
        maps.append(m)
    return maps


_NC_CACHE = {}


def kernel(**inputs):
    maps = prep_inputs(inputs)
    if "nc" not in _NC_CACHE:
        _NC_CACHE["nc"] = build_program()
    res = run_bass_kernel_spmd(_NC_CACHE["nc"], maps, core_ids=list(range(8)))
    out = np.stack([np.ascontiguousarray(r["outT"].T) for r in res.results]).astype(np.float32)
    return out
```
